# Optimizing a Trainium2 kernel written in Bass

```python
import math
import numpy as np
import jax
import jax.numpy as jnp
from jax import lax

D_MODEL = 1024
BATCH = 8
SEQ = 4096
DEPTH = 2

GRID_W = 64
CTX_LEN = 256
Q_BLOCK = 128
ROPE_BASE = 10000.0
EPS = 1e-6

NA_HEADS = 4
NA_DIM = 64
NA_KH = 8
NA_KW = 16
DF_HEADS = 4
DF_QK = 32
DF_V = 2 * DF_QK
GQ_HEADS = 4
GQ_KV_HEADS = 2
GQ_DIM = 64
ML_HEADS = 4
ML_NOPE = 64
ML_ROPE = 32
ML_V = 64
ML_Q_RANK = 256
ML_KV_RANK = 128
N_BRANCH = 4
BRANCH_W = 256
N_EXPERTS = 32
TOP_K = 4
D_FF = D_MODEL
SWIGLU_LIMIT = 7.0
SWIGLU_ALPHA = 1.702
DEEPNORM_ALPHA = (2 * DEPTH) ** 0.25
DEEPNORM_BETA = (8 * DEPTH) ** -0.25

IN_SIZES = (NA_HEADS * NA_DIM, NA_HEADS * NA_DIM, NA_HEADS * NA_DIM,
            DF_HEADS * 2 * DF_QK, DF_HEADS * 2 * DF_QK, DF_HEADS * DF_V,
            GQ_HEADS * GQ_DIM, GQ_KV_HEADS * GQ_DIM, GQ_KV_HEADS * GQ_DIM,
            ML_Q_RANK, ML_KV_RANK, ML_ROPE,
            N_BRANCH * D_MODEL)
IN_WIDTH = sum(IN_SIZES)
IN_OFFSETS = tuple(int(o) for o in np.cumsum(IN_SIZES)[:-1])

kernel_name = 'hybrid_gated_mixer_moe_trunk'


def layer_norm(x, g, b):
    xf = x.astype(jnp.float32)
    mu = jnp.mean(xf, axis=-1, keepdims=True)
    var = jnp.mean(jnp.square(xf - mu), axis=-1, keepdims=True)
    return ((xf - mu) * lax.rsqrt(var + EPS) * g + b).astype(x.dtype)


def rms_norm(x, g):
    xf = x.astype(jnp.float32)
    return (xf * lax.rsqrt(jnp.mean(jnp.square(xf), axis=-1, keepdims=True) + EPS) * g).astype(x.dtype)


def rope_1d(x, pos):
    n = x.shape[-1]
    inv_freq = ROPE_BASE ** (-jnp.arange(0, n, 2, dtype=jnp.float32) / n)
    ang = pos.astype(jnp.float32)[:, None] * inv_freq[None, :]
    cos, sin = jnp.cos(ang).astype(x.dtype), jnp.sin(ang).astype(x.dtype)
    x1, x2 = x[..., : n // 2], x[..., n // 2:]
    return jnp.concatenate([x1 * cos - x2 * sin, x2 * cos + x1 * sin], axis=-1)


def rope_2d(x, pos):
    rows, cols = pos
    h = x.shape[-1] // 2
    return jnp.concatenate([rope_1d(x[..., :h], rows), rope_1d(x[..., h:], cols)], axis=-1)


def to_heads(t, n):
    b, s, w = t.shape
    return t.reshape(b, s, n, w // n).transpose(0, 2, 1, 3)


def from_heads(t):
    b, h, s, d = t.shape
    return t.transpose(0, 2, 1, 3).reshape(b, s, h * d)


def softmax_f32(s, dtype):
    return jax.nn.softmax(s.astype(jnp.float32), axis=-1).astype(dtype)


def sdpa(q, k, v):
    s = jnp.einsum('bgrqd,bgkd->bgrqk', q, k) * (q.shape[-1] ** -0.5)
    return jnp.einsum('bgrqk,bgkd->bgrqd', softmax_f32(s, v.dtype), v)


def mha(q, k, v):
    return sdpa(q[:, :, None], k, v)[:, :, 0]


def group_queries(q):
    b, h, s, d = q.shape
    return q.reshape(b, GQ_KV_HEADS, h // GQ_KV_HEADS, s, d)


def ungroup(o):
    b, g, r, s, d = o.shape
    return o.reshape(b, g * r, s, d)


def diff_attend(q1, q2, k1, k2, v, lam):
    scale = q1.shape[-1] ** -0.5
    a1 = softmax_f32(jnp.einsum('bhqd,bhkd->bhqk', q1, k1) * scale, jnp.float32)
    a2 = softmax_f32(jnp.einsum('bhqd,bhkd->bhqk', q2, k2) * scale, jnp.float32)
    return jnp.einsum('bhqk,bhkd->bhqd', (a1 - lam * a2).astype(v.dtype), v)


def sweep_query_blocks(fn, qs):
    s = qs[0].shape[-2]
    nb = s // Q_BLOCK

    def to_blocks(q):
        return jnp.moveaxis(q.reshape(q.shape[:-2] + (nb, Q_BLOCK, q.shape[-1])), -3, 0)

    out = lax.map(lambda blk: fn(*blk), tuple(to_blocks(q) for q in qs))
    out = jnp.moveaxis(out, 0, -3)
    return out.reshape(out.shape[:-3] + (s, out.shape[-1]))


def neighbourhood_attend(q, k, v, k_ctx, v_ctx, rpb, rows):
    b, h, s, d = q.shape
    kh, kw = min(NA_KH, rows), NA_KW
    n_keys = kh * kw
    scale = d ** -0.5
    col = jnp.arange(GRID_W, dtype=jnp.int32)
    col_start = jnp.clip(col - kw // 2, 0, GRID_W - kw)
    row_start = jnp.clip(jnp.arange(rows, dtype=jnp.int32) - kh // 2, 0, rows - kh)
    di = jnp.repeat(jnp.arange(kh, dtype=jnp.int32), kw)
    dj = jnp.tile(jnp.arange(kw, dtype=jnp.int32), kh)
    key_col = col_start[:, None] + dj[None, :]
    col_bias_idx = key_col - col[:, None] + (NA_KW - 1)
    q_rows = q.reshape(b, h, rows, GRID_W, d).transpose(2, 0, 1, 3, 4)

    def one_row(args):
        qr, r, rs = args
        key_row = rs + di
        idx = key_row[None, :] * GRID_W + key_col
        kg = jnp.take(k, idx, axis=2)
        vg = jnp.take(v, idx, axis=2)
        row_bias_idx = (key_row - r + (NA_KH - 1))[None, :]
        bias = rpb[:, row_bias_idx, col_bias_idx]
        s_loc = jnp.einsum('bhqd,bhqkd->bhqk', qr, kg) * scale + bias[None]
        s_ctx = jnp.einsum('bhqd,bhkd->bhqk', qr, k_ctx) * scale
        p = softmax_f32(jnp.concatenate([s_loc, s_ctx], axis=-1), v.dtype)
        return (jnp.einsum('bhqk,bhqkd->bhqd', p[..., :n_keys], vg)
                + jnp.einsum('bhqk,bhkd->bhqd', p[..., n_keys:], v_ctx))

    out = lax.map(one_row, (q_rows, jnp.arange(rows, dtype=jnp.int32), row_start))
    return out.transpose(1, 2, 0, 3, 4).reshape(b, h, s, d)


def project_tokens(h, pos, w_in, gq_qnorm, gq_knorm, ml_qa_norm, ml_wq_b, ml_kva_norm, ml_wkv_b):
    rot = (lambda t: rope_2d(t, pos)) if pos is not None else (lambda t: t)
    (na_q, na_k, na_v, df_q, df_k, df_v, gq_q, gq_k, gq_v,
     ml_qa, ml_kva, ml_kr, gates) = jnp.split(h @ w_in, IN_OFFSETS, axis=-1)
    b, s, _ = h.shape
    df_q, df_k = to_heads(df_q, DF_HEADS), to_heads(df_k, DF_HEADS)
    ml_q = to_heads(rms_norm(ml_qa, ml_qa_norm) @ ml_wq_b, ML_HEADS)
    ml_kv = to_heads(rms_norm(ml_kva, ml_kva_norm) @ ml_wkv_b, ML_HEADS)
    k_rope = rot(ml_kr[:, None])
    return {
        'na_q': to_heads(na_q, NA_HEADS), 'na_k': to_heads(na_k, NA_HEADS), 'na_v': to_heads(na_v, NA_HEADS),
        'df_q1': rot(df_q[..., :DF_QK]), 'df_q2': rot(df_q[..., DF_QK:]),
        'df_k1': rot(df_k[..., :DF_QK]), 'df_k2': rot(df_k[..., DF_QK:]),
        'df_v': to_heads(df_v, DF_HEADS),
        'gq_q': rot(rms_norm(to_heads(gq_q, GQ_HEADS), gq_qnorm)),
        'gq_k': rot(rms_norm(to_heads(gq_k, GQ_KV_HEADS), gq_knorm)),
        'gq_v': to_heads(gq_v, GQ_KV_HEADS),
        'ml_q': jnp.concatenate([ml_q[..., :ML_NOPE], rot(ml_q[..., ML_NOPE:])], axis=-1),
        'ml_k': jnp.concatenate([ml_kv[..., :ML_NOPE],
                                 jnp.broadcast_to(k_rope, ml_kv.shape[:-1] + (ML_ROPE,))], axis=-1),
        'ml_v': ml_kv[..., ML_NOPE:],
        'gates': gates.reshape(b, s, N_BRANCH, D_MODEL),
    }


def merge_branches(outs, gates, w_branch, w_out):
    y = None
    for i, o in enumerate(outs):
        term = jax.nn.sigmoid(gates[:, :, i]) * (from_heads(o) @ w_branch[i])
        y = term if y is None else y + term
    return y @ w_out


def latent_mix(lat, cx, rows, lam, lam_init, rpb, subln, w_branch, w_out):
    cat = lambda a, b: jnp.concatenate([a, b], axis=2)
    o_na = neighbourhood_attend(lat['na_q'], lat['na_k'], lat['na_v'], cx['na_k'], cx['na_v'], rpb, rows)
    k1, k2, dv = cat(cx['df_k1'], lat['df_k1']), cat(cx['df_k2'], lat['df_k2']), cat(cx['df_v'], lat['df_v'])
    o_df = sweep_query_blocks(lambda a, b: diff_attend(a, b, k1, k2, dv, lam), (lat['df_q1'], lat['df_q2']))
    o_df = rms_norm(o_df, subln) * (1.0 - lam_init)
    gk, gv = cat(cx['gq_k'], lat['gq_k']), cat(cx['gq_v'], lat['gq_v'])
    o_gq = ungroup(sweep_query_blocks(lambda a: sdpa(a, gk, gv), (group_queries(lat['gq_q']),)))
    mk, mv = cat(cx['ml_k'], lat['ml_k']), cat(cx['ml_v'], lat['ml_v'])
    o_ml = sweep_query_blocks(lambda a: mha(a, mk, mv), (lat['ml_q'],))
    return merge_branches((o_na, o_df, o_gq, o_ml), lat['gates'], w_branch, w_out)


def context_mix(cx, lam, lam_init, subln, w_branch, w_out):
    o_na = mha(cx['na_q'], cx['na_k'], cx['na_v'])
    o_df = rms_norm(diff_attend(cx['df_q1'], cx['df_q2'], cx['df_k1'], cx['df_k2'], cx['df_v'], lam), subln) * (1.0 - lam_init)
    o_gq = ungroup(sdpa(group_queries(cx['gq_q']), cx['gq_k'], cx['gq_v']))
    o_ml = mha(cx['ml_q'], cx['ml_k'], cx['ml_v'])
    return merge_branches((o_na, o_df, o_gq, o_ml), cx['gates'], w_branch, w_out)


def moe(h, router_w, router_b, w1, b1, w2, b2):
    logits = (h @ router_w + router_b).astype(jnp.float32)
    top_val, top_idx = lax.top_k(logits, TOP_K)
    top_w = jax.nn.softmax(top_val, axis=-1)
    combine = jnp.einsum('nk,nke->ne', top_w, jax.nn.one_hot(top_idx, N_EXPERTS, dtype=jnp.float32)).astype(h.dtype)

    def expert_step(acc, xs):
        w1e, b1e, w2e, b2e, ge = xs
        gu = h @ w1e + b1e
        gate = jnp.minimum(gu[:, :D_FF], SWIGLU_LIMIT)
        up = jnp.clip(gu[:, D_FF:], -SWIGLU_LIMIT, SWIGLU_LIMIT)
        y = ((up + 1.0) * gate * jax.nn.sigmoid(SWIGLU_ALPHA * gate)) @ w2e + b2e
        return acc + ge[:, None] * y, None

    out, _ = lax.scan(expert_step, jnp.zeros_like(h), (w1, b1, w2, b2, combine.T))
    return out


def setup_inputs(seed: int = 0) -> dict:
    key = jax.random.key(seed)
    ks = iter(jax.random.split(key, 32))
    L, D, f32 = DEPTH, D_MODEL, jnp.float32

    def normal(shape, scale):
        return scale * jax.random.normal(next(ks), shape, f32)

    def gain(shape):
        return 1.0 + normal(shape, 0.1)

    return {
        'x': normal((BATCH, SEQ, D), 1.0),
        'c': normal((BATCH, D), 1.0),
        'ctx': normal((BATCH, CTX_LEN, D), 1.0),
        'c_ctx': normal((D,), 1.0),
        'w_ada': normal((L, D, 6 * D), 0.5 * D ** -0.5),
        'b_ada': normal((L, 6 * D), 0.02),
        'w_in': normal((L, D, IN_WIDTH), D ** -0.5),
        'na_rpb': normal((L, NA_HEADS, 2 * NA_KH - 1, 2 * NA_KW - 1), 0.1),
        'df_lam': normal((L, 4, DF_QK), 0.1),
        'df_subln': gain((L, DF_V)),
        'gq_qnorm': gain((L, GQ_DIM)),
        'gq_knorm': gain((L, GQ_DIM)),
        'ml_qa_norm': gain((L, ML_Q_RANK)),
        'ml_wq_b': normal((L, ML_Q_RANK, ML_HEADS * (ML_NOPE + ML_ROPE)), ML_Q_RANK ** -0.5),
        'ml_kva_norm': gain((L, ML_KV_RANK)),
        'ml_wkv_b': normal((L, ML_KV_RANK, ML_HEADS * (ML_NOPE + ML_V)), ML_KV_RANK ** -0.5),
        'w_branch': normal((L, N_BRANCH, BRANCH_W, D), DEEPNORM_BETA * BRANCH_W ** -0.5),
        'w_out': normal((L, D, D), DEEPNORM_BETA * D ** -0.5),
        'ln1_g': gain((L, D)),
        'ln1_b': normal((L, D), 0.02),
        'ln2_g': gain((L, D)),
        'ln2_b': normal((L, D), 0.02),
        'router_w': normal((L, D, N_EXPERTS), D ** -0.5),
        'router_b': normal((L, N_EXPERTS), 0.01),
        'exp_w1': normal((L, N_EXPERTS, D, 2 * D_FF), D ** -0.5),
        'exp_b1': normal((L, N_EXPERTS, 2 * D_FF), 0.02),
        'exp_w2': normal((L, N_EXPERTS, D_FF, D), DEEPNORM_BETA * D_FF ** -0.5),
        'exp_b2': normal((L, N_EXPERTS, D), 0.02),
    }


def reference(x, c, ctx, c_ctx, w_ada, b_ada, w_in, na_rpb, df_lam, df_subln, gq_qnorm, gq_knorm,
              ml_qa_norm, ml_wq_b, ml_kva_norm, ml_wkv_b, w_branch, w_out, ln1_g, ln1_b, ln2_g, ln2_b,
              router_w, router_b, exp_w1, exp_b1, exp_w2, exp_b2):
    B, S, D = x.shape
    rows = S // GRID_W
    t = jnp.arange(S, dtype=jnp.int32)
    pos = (t // GRID_W, t % GRID_W)
    xl, xc = x, ctx
    for l in range(DEPTH):
        last = l == DEPTH - 1
        mod_l = jax.nn.silu(c) @ w_ada[l] + b_ada[l]
        mod_c = jax.nn.silu(c_ctx) @ w_ada[l] + b_ada[l]
        sh1, sc1, g1, sh2, sc2, g2 = [m[:, None, :] for m in jnp.split(mod_l, 6, axis=-1)]
        csh1, csc1, cg1, csh2, csc2, cg2 = jnp.split(mod_c, 6, axis=-1)
        lam_init = 0.8 - 0.6 * math.exp(-0.3 * l)
        lp = df_lam[l].astype(jnp.float32)
        lam = jnp.exp(jnp.sum(lp[0] * lp[1])) - jnp.exp(jnp.sum(lp[2] * lp[3])) + lam_init
        proj_w = (w_in[l], gq_qnorm[l], gq_knorm[l], ml_qa_norm[l], ml_wq_b[l], ml_kva_norm[l], ml_wkv_b[l])
        lat = project_tokens(xl * (1.0 + sc1) + sh1, pos, *proj_w)
        cx = project_tokens(xc * (1.0 + csc1) + csh1, None, *proj_w)
        y_l = latent_mix(lat, cx, rows, lam, lam_init, na_rpb[l], df_subln[l], w_branch[l], w_out[l])
        xl = layer_norm(DEEPNORM_ALPHA * xl + g1 * y_l, ln1_g[l], ln1_b[l])
        h_l = (xl * (1.0 + sc2) + sh2).reshape(B * S, D)
        moe_w = (router_w[l], router_b[l], exp_w1[l], exp_b1[l], exp_w2[l], exp_b2[l])
        if last:
            y2 = moe(h_l, *moe_w)
        else:
            y_c = context_mix(cx, lam, lam_init, df_subln[l], w_branch[l], w_out[l])
            xc = layer_norm(DEEPNORM_ALPHA * xc + cg1 * y_c, ln1_g[l], ln1_b[l])
            h_c = (xc * (1.0 + csc2) + csh2).reshape(-1, D)
            y2 = moe(jnp.concatenate([h_l, h_c], axis=0), *moe_w)
            xc = layer_norm(DEEPNORM_ALPHA * xc + cg2 * y2[B * S:].reshape(xc.shape), ln2_g[l], ln2_b[l])
        xl = layer_norm(DEEPNORM_ALPHA * xl + g2 * y2[: B * S].reshape(B, S, D), ln2_g[l], ln2_b[l])
    return xl
```

```python
import contextlib
import math
import numpy as np
import concourse.bass as bass
import concourse.mybir as mybir
from concourse.bass_utils import run_bass_kernel_spmd

F32 = mybir.dt.float32
BF16 = mybir.dt.bfloat16
U32 = mybir.dt.uint32
AF = mybir.ActivationFunctionType
ALU = mybir.AluOpType
AX = mybir.AxisListType

D = 1024
CTX = 256
GRID_W = 64
NEXP = 32
EPS = 1e-6
NEG = -30000.0


class Buf:
    __slots__ = ("name", "last_w", "readers", "t")

    def __init__(self, name, t=None):
        self.name = name
        self.last_w = None
        self.readers = {}
        self.t = t

    def __getitem__(self, idx):
        return self.t[idx]


class Prog:
    ENGS = ("pe", "act", "dve", "pool", "sp")

    def __init__(self, nc, stack):
        self.nc = nc
        self.stack = stack
        self.streams = {e: [] for e in self.ENGS}
        self.count = {}
        self.clock = {e: {} for e in self.ENGS}
        self.ops = []
        self.sems = {}
        self.free_sems = []
        self.fence = 0
        self.root = stack
        for e in self.ENGS:
            self.sem(e)

    def sem(self, key):
        if key not in self.sems:
            if self.free_sems:
                self.sems[key], self.count[key] = self.free_sems.pop()
            else:
                self.nsem = getattr(self, "nsem", 0) + 1
                self.sems[key] = self.root.enter_context(self.nc.semaphore("s_%d" % self.nsem))
                self.count[key] = 0
        return self.sems[key]

    def _uniq(self, name):
        self.nalloc = getattr(self, "nalloc", 0) + 1
        return "%s_%d" % (name, self.nalloc)

    def sbuf(self, name, shape, dtype):
        return Buf(name, self.stack.enter_context(self.nc.sbuf_tensor(self._uniq(name), list(shape), dtype)))

    def psum(self, name, shape, dtype=F32):
        return Buf(name, self.stack.enter_context(self.nc.psum_tensor(self._uniq(name), list(shape), dtype)))

    def dram(self, name, shape, dtype):
        return Buf(name, self.nc.dram_tensor(name, list(shape), dtype, kind="Internal").ap())

    @contextlib.contextmanager
    def scope(self):
        old = self.stack
        with contextlib.ExitStack() as st:
            self.stack = st
            try:
                yield
            finally:
                self.flush()
                self.stack = old

    def _emit(self, eng, fn, reads, writes, dma_key=None, nowaw=None):
        deps = set()
        for b in reads:
            if b.last_w is not None:
                deps.add(b.last_w)
        for b in writes:
            if b.last_w is not None and not (nowaw is not None and self.ops[b.last_w][0] == nowaw):
                deps.add(b.last_w)
            deps.update(b.readers.values())
        clk = self.clock[eng]
        waits = {}
        for d in deps:
            if d < self.fence:
                continue
            dim, val, vc = self.ops[d]
            if eng == "pe" and dim == "pe" and dma_key is None:
                continue
            if clk.get(dim, 0) >= val:
                continue
            if waits.get(dim, 0) < val:
                waits[dim] = val
            for k, v in vc.items():
                if clk.get(k, 0) < v:
                    clk[k] = v
        if dma_key is not None:
            dim = dma_key
            self.sem(dim)
            self.count[dim] += 16
        else:
            dim = eng
            self.count[dim] += 1
        val = self.count[dim]
        vc = dict(clk)
        vc[dim] = val
        oid = len(self.ops)
        self.ops.append((dim, val, vc))
        ws = set(id(b) for b in writes)
        for b in writes:
            b.last_w = oid
            b.readers = {}
        for b in reads:
            if id(b) not in ws:
                b.readers[dim] = oid
        self.streams[eng].append(([(self.sems[k], v) for k, v in waits.items()], fn, self.sems[dim], dma_key is not None))
        return oid

    def op(self, eng, fn, reads=(), writes=()):
        return self._emit(eng, fn, reads, writes)

    def dma(self, q, out_ap, in_ap, reads, writes, key):
        return self._emit(q, lambda e: e.dma_start(out=out_ap, in_=in_ap), reads, writes, dma_key=key)

    def barrier(self):
        allv = dict((k, v) for k, v in self.count.items() if v > 0)
        for e in self.ENGS:
            clk = self.clock[e]
            waits = [(self.sems[k], v) for k, v in allv.items() if clk.get(k, 0) < v]
            for k, v in allv.items():
                clk[k] = max(clk.get(k, 0), v)
            if waits:
                self.streams[e].append((waits, None, None, False))
        self.fence = len(self.ops)

    def flush(self):
        nc = self.nc
        hand = {"pe": "tensor", "act": "scalar", "dve": "vector", "pool": "gpsimd", "sp": "sync"}
        self.barrier()
        if not any(self.streams[e] for e in self.ENGS):
            return
        streams = self.streams
        self.streams = {e: [] for e in self.ENGS}
        with nc.Block() as block:
            for e in self.ENGS:
                stream = streams[e]

                def body(eng, stream=stream):
                    for waits, fn, sem, is_dma in stream:
                        for s, v in waits:
                            eng.wait_ge(s, v)
                        if fn is not None:
                            fn(eng).then_inc(sem, 16 if is_dma else 1)

                getattr(block, hand[e])(body)
        for k in [k for k in self.sems if k not in self.ENGS]:
            self.free_sems.append((self.sems.pop(k), self.count.pop(k)))
            for e in self.ENGS:
                self.clock[e].pop(k, None)


def rope_tables(S):
    T = CTX + S
    tab = np.zeros((T, 192), np.float32)
    tab[:, 0:32] = 1.0
    tab[:, 64:128] = 1.0
    t = np.arange(S)
    pos = (t // GRID_W, t % GRID_W)
    for n, c0, s0 in ((32, 0, 32), (64, 64, 128)):
        m = n // 2
        q = m // 2
        inv = (10000.0 ** (-np.arange(0, m, 2, dtype=np.float32) / m)).astype(np.float32)
        for part in range(2):
            ang = pos[part].astype(np.float32)[:, None] * inv[None, :]
            cs, sn = np.cos(ang).astype(np.float32), np.sin(ang).astype(np.float32)
            o = part * m
            tab[CTX:, c0 + o:c0 + o + q] = cs
            tab[CTX:, c0 + o + q:c0 + o + 2 * q] = cs
            tab[CTX:, s0 + o:s0 + o + q] = -sn
            tab[CTX:, s0 + o + q:s0 + o + 2 * q] = sn
    return tab


def na_structure(S):
    rows = S // GRID_W
    kh, kw = min(8, rows), 16
    cfgs = {}
    cfg_list = []
    per_q = []
    c = np.arange(GRID_W)
    cs = np.clip(c - kw // 2, 0, GRID_W - kw)
    for qt in range(S // 128):
        r = np.array([2 * qt, 2 * qt + 1])
        rs = np.clip(r - kh // 2, 0, rows - kh)
        lo, hi = rs.min(), rs.max() + kh - 1
        lst = []
        for kt in range(lo // 2, hi // 2 + 1):
            key = (kt - qt, int(rs[0] - r[0]), int(rs[1] - r[1]))
            if key not in cfgs:
                krl, kc = np.divmod(np.arange(128), 64)
                rl, cc = np.divmod(np.arange(128), 64)
                kr = 2 * kt + krl[:, None]
                rr = 2 * qt + rl[None, :]
                rsq = rs[rl][None, :]
                valid = (kr >= rsq) & (kr < rsq + kh) & (kc[:, None] >= cs[cc][None, :]) & (kc[:, None] < cs[cc][None, :] + kw)
                a = np.clip(kr - rr + 7, 0, 14)
                b = np.clip(kc[:, None] - cc[None, :] + 15, 0, 30)
                cfgs[key] = len(cfg_list)
                cfg_list.append((a, b, np.where(valid, 0.0, NEG).astype(np.float32)))
            lst.append((kt, cfgs[key]))
        per_q.append(lst)
    return per_q, cfg_list


def build(S, L, NE=NEXP, dbg=False):
    T = CTX + S
    NT = T // 128
    NCT = CTX // 128
    per_q, cfg_list = na_structure(S)
    NCFG = len(cfg_list)
    ALPHA = (2 * L) ** 0.25

    nc = bass.Bass("TRN2", target_bir_lowering=False)

    def din(name, shape):
        return nc.dram_tensor(name, list(shape), F32, kind="ExternalInput").ap()

    x_in = din("x", [S, D])
    ctx_in = din("ctx", [CTX, D])
    c2_in = din("c2", [2, D])
    w_ada = din("w_ada", [L, D, 6 * D])
    b_ada = din("b_ada", [L, 6 * D])
    w_in = din("w_in", [L, D, 6560])
    na_bias = din("na_bias", [L, NCFG, 4, 128, 128])
    na_mask = din("na_mask", [NCFG, 128, 128])
    df_lam = din("df_lam", [L, 128])
    df_subln = din("df_subln", [L, 64])
    gq_qnorm = din("gq_qnorm", [L, 64])
    gq_knorm = din("gq_knorm", [L, 64])
    ml_qa_norm = din("ml_qa_norm", [L, 256])
    ml_wq_b = din("ml_wq_b", [L, 256, 384])
    ml_kva_norm = din("ml_kva_norm", [L, 128])
    ml_wkv_b = din("ml_wkv_b", [L, 128, 512])
    w_branch = din("w_branch", [L, 1024, D])
    w_out = din("w_out", [L, D, D])
    ln1_g = din("ln1_g", [L, D])
    ln1_b = din("ln1_b", [L, D])
    ln2_g = din("ln2_g", [L, D])
    ln2_b = din("ln2_b", [L, D])
    router_w = din("router_w", [L, D, NE])
    router_b = din("router_b", [L, NE])
    exp_w1 = din("exp_w1", [L, NE, D, 2 * D])
    exp_b1 = din("exp_b1", [L, NE, 2 * D])
    exp_w2 = din("exp_w2", [L, NE, D, D])
    exp_b2 = din("exp_b2", [L, NE, D])
    idn_in = din("idn", [128, 128])
    rope_in = din("rope", [T, 192])
    NSLOT = (4 * T + 511) // 512 + NE
    NPOS = NSLOT * 512
    tri_in = din("tri", [128, 128])
    sst_in = din("slotstart", [128, NSLOT])
    iok_in = din("iotak", [128, 8])
    ioe_in = din("iotae", [128, NE])
    out_ap = nc.dram_tensor("out", [S, D], F32, kind="ExternalOutput").ap()

    with contextlib.ExitStack() as root:
        P = Prog(nc, root)
        MOD = P.dram("MOD", [2, 128, 6 * D], F32)
        xA = P.dram("xA", [T, D], F32)
        xB = P.dram("xB", [T, D], F32)
        hTs = P.dram("hTs", [NT, 128, 8, 128], BF16)
        qT = {"na": P.dram("qT_na", [4, 64, T], BF16), "df": P.dram("qT_df", [4, 64, T], BF16),
              "gq": P.dram("qT_gq", [4, 64, T], BF16), "ml": P.dram("qT_ml", [384, T], BF16)}
        kT = {"na": P.dram("kT_na", [4, 64, T], BF16), "df": P.dram("kT_df", [4, 64, T], BF16),
              "gq": P.dram("kT_gq", [2, 64, T], BF16), "ml": P.dram("kT_ml", [384, T], BF16)}
        vS = {"na": P.dram("v_na", [T, 4, 128], BF16), "df": P.dram("v_df", [T, 4, 128], BF16),
              "gq": P.dram("v_gq", [T, 2, 128], BF16), "ml": P.dram("v_ml", [T, 4, 128], BF16)}
        oT = P.dram("oT", [4, 4, 64, T], BF16)
        h2bS = P.dram("h2bS", [T, D], BF16)
        mskS = P.dram("mskS", [T, NE], F32)
        rankS = P.dram("rankS", [T, NE], F32)
        cntS = P.dram("cntS", [128, NE], F32)
        XsT = nc.dram_tensor("Xs", [NPOS, D], BF16, kind="Internal").ap()
        YsT = nc.dram_tensor("Ys", [NPOS, D], F32, kind="Internal").ap()
        XsB = [Buf("Xs%d" % i, XsT[i * 512:(i + 1) * 512, :]) for i in range(NSLOT)]
        YsB = [Buf("Ys%d" % i, YsT[i * 512:(i + 1) * 512, :]) for i in range(NSLOT)]
        combS = P.dram("combS", [T, 128], F32)
        dbgbufs = {}

        idf = P.sbuf("idf", [128, 128], F32)
        idb = P.sbuf("idb", [128, 128], BF16)
        P.dma("sp", idf[:], idn_in, [], [idf], "c_idf")
        P.op("dve", lambda e: e.tensor_copy(out=idb[:], in_=idf[:]), [idf], [idb])

        with P.scope():
            zt = P.sbuf("zt", [128, 4, D], BF16)
            P.op("dve", lambda e: e.memset(zt[:], 0.0), [], [zt])
            for i in range(NSLOT):
                P.dma("sp", XsB[i].t.rearrange("(n p) d -> p n d", p=128), zt[:], [zt], [XsB[i]], "z_xs")

        def MM(out, lhsT, rhs, start, stop, R, W):
            P.op("pe", lambda e: e.matmul(out=out, lhsT=lhsT, rhs=rhs, start=start, stop=stop), R, W)

        def TRB(out, in_, R, W):
            P.op("pe", lambda e: e.transpose(out=out, in_=in_, identity=idb[:]), list(R) + [idb], W)

        def TRF(out, in_, R, W):
            P.op("pe", lambda e: e.transpose(out=out, in_=in_, identity=idf[:]), list(R) + [idf], W)

        def TT(eng, out, in0, in1, op, R, W):
            P.op(eng, lambda e: e.tensor_tensor(out=out, in0=in0, in1=in1, op=op), R, W)

        def TS(eng, out, in0, s1, op0, R, W, s2=None, op1=None):
            if op1 is None:
                P.op(eng, lambda e: e.tensor_scalar(out=out, in0=in0, scalar1=s1, scalar2=None, op0=op0), R, W)
            else:
                P.op(eng, lambda e: e.tensor_scalar(out=out, in0=in0, scalar1=s1, scalar2=s2, op0=op0, op1=op1), R, W)

        def STT(out, in0, sc, in1, op0, op1, R, W):
            P.op("dve", lambda e: e.scalar_tensor_tensor(out=out, in0=in0, scalar=sc, in1=in1, op0=op0, op1=op1), R, W)

        def ACT(out, in_, func, R, W, scale=1.0, bias=0.0, accum=None):
            if accum is None:
                P.op("act", lambda e: e.activation(out=out, in_=in_, func=func, bias=bias, scale=scale), R, W)
            else:
                P.op("act", lambda e: e.activation(out=out, in_=in_, func=func, bias=bias, scale=scale, accum_out=accum), R, W)

        def CP(eng, out, in_, R, W):
            if eng == "act":
                P.op("act", lambda e: e.copy(out=out, in_=in_), R, W)
            else:
                P.op(eng, lambda e: e.tensor_copy(out=out, in_=in_), R, W)

        def MEMSET(eng, ap, val, W):
            P.op(eng, lambda e: e.memset(ap, val), [], W)

        def rstd_from(ss, scale, G):
            ACT(ss[:, 0:G], ss[:, 0:G], AF.Sqrt, [ss], [ss], scale=scale, bias=EPS)
            P.op("dve", lambda e: e.reciprocal(out=ss[:, 0:G], in_=ss[:, 0:G]), [ss], [ss])

        def layer_norm(xt, tmp, gam, bet, st6, mv, outb, geng="pool"):
            P.op("dve", lambda e: e.bn_stats(out=st6[:, 0, :], in_=xt[:, 0:512]), [xt], [st6])
            P.op("dve", lambda e: e.bn_stats(out=st6[:, 1, :], in_=xt[:, 512:1024]), [xt], [st6])
            P.op("dve", lambda e: e.bn_aggr(out=mv[:, 0:2], in_=st6[:].rearrange("p a b -> p (a b)")), [st6], [mv])
            ACT(mv[:, 1:2], mv[:, 1:2], AF.Sqrt, [mv], [mv], scale=1.0, bias=EPS)
            P.op("dve", lambda e: e.reciprocal(out=mv[:, 1:2], in_=mv[:, 1:2]), [mv], [mv])
            TS("dve", tmp[:], xt[:], mv[:, 0:1], ALU.subtract, [xt, mv], [tmp], s2=mv[:, 1:2], op1=ALU.mult)
            TT(geng, tmp[:], tmp[:], gam[:], ALU.mult, [tmp, gam], [tmp])
            TT("dve", outb[:], tmp[:], bet[:], ALU.add, [tmp, bet], [outb])

        def stage0(l):
            with P.scope():
                c2s = P.sbuf("c2s", [128, D], F32)
                MEMSET("dve", c2s[:], 0.0, [c2s])
                P.dma("sp", c2s[0:2, :], c2_in, [], [c2s], "s0_c2")
                ACT(c2s[:], c2s[:], AF.Silu, [c2s], [c2s])
                pt = P.psum("s0_pt", [128, 8, 128], F32)
                cT = P.sbuf("cT", [128, 8, 128], F32)
                for k in range(8):
                    TRF(pt[:, k, :], c2s[:, k * 128:(k + 1) * 128], [c2s], [pt])
                CP("dve", cT[:], pt[:], [pt], [cT])
                ones = P.sbuf("s0_ones", [128, 128], F32)
                MEMSET("dve", ones[:], 1.0, [ones])
                rep = [P.sbuf("rep%d" % s, [128, 8, 128], BF16) for s in range(2)]
                for s in range(2):
                    for k in range(8):
                        TS("dve", rep[s][:, k, :], ones[:], cT[:, k, s:s + 1], ALU.mult, [ones, cT], [rep[s]])
                bb = P.sbuf("s0_bb", [128, 6 * D], F32)
                P.dma("sp", bb[:], b_ada[l].partition_broadcast(128), [], [bb], "s0_bb")
                wa = [P.sbuf("s0_wa%d" % i, [128, 8, 512], BF16) for i in range(2)]
                pm = [P.psum("s0_pm%d" % i, [128, 512], F32) for i in range(2)]
                mo = [P.sbuf("s0_mo%d" % i, [128, 512], F32) for i in range(2)]
                n = 0
                for j in range(12):
                    w = wa[j % 2]
                    P.dma("pool", w[:], w_ada[l][:, j * 512:(j + 1) * 512].rearrange("(k p) n -> p k n", p=128), [], [w], "s0_wa%d" % (j % 2))
                    for s in range(2):
                        pp, m = pm[n % 2], mo[n % 2]
                        n += 1
                        for k in range(8):
                            MM(pp[:], rep[s][:, k, :], w[:, k, :], k == 0, k == 7, [rep[s], w], [pp])
                        if j in (2, 3, 8, 9):
                            STT(m[:], pp[:], 1.0, bb[:, j * 512:(j + 1) * 512], ALU.add, ALU.add, [pp, bb], [m])
                        else:
                            TT("dve", m[:], pp[:], bb[:, j * 512:(j + 1) * 512], ALU.add, [pp, bb], [m])
                        P.dma("sp", MOD[s, :, j * 512:(j + 1) * 512], m[:], [m], [MOD], "s0_mo%d" % ((n - 1) % 2))

        def rope(eng, X, G, n, cos, sin, t1, t2, out, R, W, ropeb):
            q = n // 4
            v1 = t1[:, 0:G * n].rearrange("p (g d) -> p g d", g=G)
            v2 = t2[:, 0:G * n].rearrange("p (g d) -> p g d", g=G)
            TT(eng, v1, X, cos.unsqueeze(1).broadcast_to([128, G, n]), ALU.mult, list(R) + [ropeb], [t1])
            for part in range(2):
                for half in range(2):
                    o = part * 2 * q + half * q
                    so = part * 2 * q + (1 - half) * q
                    TT(eng, v2[:, :, o:o + q], X[:, :, so:so + q], sin[:, o:o + q].unsqueeze(1).broadcast_to([128, G, q]),
                       ALU.mult, list(R) + [ropeb], [t2])
            TT(eng, out, v1, v2, ALU.add, [t1, t2], W)

        def stage1(l, last):
            with P.scope():
                win = P.sbuf("win", [128, 8, 2464], BF16)
                wsrc = w_in[l][:, 0:2464].rearrange("(k p) n -> p k n", p=128)
                for k in range(0, 8, 2):
                    P.dma("pool", win[:, k:k + 2, :], wsrc[:, k:k + 2, :], [], [win], "s1_win")
                wqb = P.sbuf("wqb", [128, 2, 384], BF16)
                P.dma("pool", wqb[:], ml_wq_b[l].rearrange("(k p) n -> p k n", p=128), [], [wqb], "s1_wqb")
                wkvb = P.sbuf("wkvb", [128, 512], BF16)
                P.dma("pool", wkvb[:], ml_wkv_b[l], [], [wkvb], "s1_wkvb")
                gqn = P.sbuf("gqn", [128, 64], F32)
                gkn = P.sbuf("gkn", [128, 64], F32)
                qan = P.sbuf("qan", [128, 256], F32)
                kvan = P.sbuf("kvan", [128, 128], F32)
                P.dma("sp", gqn[:], gq_qnorm[l].partition_broadcast(128), [], [gqn], "s1_c0")
                P.dma("sp", gkn[:], gq_knorm[l].partition_broadcast(128), [], [gkn], "s1_c1")
                P.dma("sp", qan[:], ml_qa_norm[l].partition_broadcast(128), [], [qan], "s1_c2")
                P.dma("sp", kvan[:], ml_kva_norm[l].partition_broadcast(128), [], [kvan], "s1_c3")
                A1 = [P.sbuf("A1_%d" % s, [128, D], F32) for s in range(2)]
                B1 = [P.sbuf("B1_%d" % s, [128, D], F32) for s in range(2)]
                for s in range(2):
                    P.dma("sp", A1[s][:], MOD[s, :, 1024:2048], [MOD], [A1[s]], "s1_A%d" % s)
                    P.dma("sp", B1[s][:], MOD[s, :, 0:1024], [MOD], [B1[s]], "s1_B%d" % s)
                pj = [P.psum("pj%d" % i, [128, 512], F32) for i in range(5)]
                ptr = [P.psum("ptr%d" % i, [128, 8, 128], BF16) for i in range(2)]
                xs = [P.sbuf("s1_xs%d" % i, [128, D], F32) for i in range(2)]
                rp = [P.sbuf("s1_rp%d" % i, [128, 192], F32) for i in range(2)]
                def mkset(i):
                    d = {}
                    d["hf"] = P.sbuf("s1_hf%d" % i, [128, D], F32)
                    d["hb"] = P.sbuf("s1_hb%d" % i, [128, D], BF16)
                    d["hT"] = P.sbuf("s1_hT%d" % i, [128, 8, 128], BF16)
                    d["pjs"] = P.sbuf("pjs%d" % i, [128, 2464], F32)
                    d["t1"] = P.sbuf("s1_t1%d" % i, [128, 256], F32)
                    d["t2"] = P.sbuf("s1_t2%d" % i, [128, 256], F32)
                    d["t3"] = P.sbuf("s1_t3%d" % i, [128, 256], F32)
                    d["ss"] = P.sbuf("s1_ss%d" % i, [128, 8], F32)
                    d["stq"] = P.sbuf("stq%d" % i, [128, 384], BF16)
                    d["stk"] = P.sbuf("stk%d" % i, [128, 384], BF16)
                    d["vt"] = {"na": P.sbuf("vt_na%d" % i, [128, 4, 128], BF16), "df": P.sbuf("vt_df%d" % i, [128, 4, 128], BF16),
                               "gq": P.sbuf("vt_gq%d" % i, [128, 2, 128], BF16), "ml": P.sbuf("vt_ml%d" % i, [128, 4, 128], BF16)}
                    for b in d["vt"].values():
                        MEMSET("pool", b[:], 1.0, [b])
                    d["nrm"] = P.sbuf("s1_nrm%d" % i, [128, 256], BF16)
                    d["nT"] = P.sbuf("s1_nT%d" % i, [128, 3, 128], BF16)
                    d["mlq"] = P.sbuf("mlq%d" % i, [128, 384], F32)
                    d["mlkv"] = P.sbuf("mlkv%d" % i, [128, 512], F32)
                    d["krr"] = P.sbuf("krr%d" % i, [128, 32], F32)
                    d["junk"] = P.sbuf("s1_junk%d" % i, [128, 256], F32)
                    return d
                sets = [mkset(0), mkset(1)]
                tsbs = [P.sbuf("tsb%d" % i, [128, 8, 128], BF16) for i in range(3)]

                def load_x(t):
                    b = xs[t % 2]
                    if l == 0:
                        src = ctx_in[t * 128:(t + 1) * 128, :] if t < NCT else x_in[(t - NCT) * 128:(t - NCT + 1) * 128, :]
                        P.dma("sp", b[:], src, [], [b], "s1_x%d" % (t % 2))
                    else:
                        P.dma("sp", b[:], xB[t * 128:(t + 1) * 128, :], [xB], [b], "s1_x%d" % (t % 2))
                    P.dma("sp", rp[t % 2][:], rope_in[t * 128:(t + 1) * 128, :], [], [rp[t % 2]], "s1_r%d" % (t % 2))

                def trans_store(stg, ncol, dst_views, tag):
                    nch = ncol // 128
                    pb = ptr[trans_store.n % 2]
                    tsb = tsbs[trans_store.n % 3]
                    tkey = "s1_ts%d" % (trans_store.n % 3)
                    trans_store.n += 1
                    for c in range(nch):
                        TRB(pb[:, c, :], stg[:, c * 128:(c + 1) * 128], [stg], [pb])
                    CP("act", tsb[:, 0:nch, :], pb[:, 0:nch, :], [pb], [tsb])
                    for c in range(nch):
                        P.dma("sp", dst_views[c][0], tsb[:, c, :], [tsb], [dst_views[c][1]], tkey)
                trans_store.n = 0

                load_x(0)
                for t in range(NT):
                    if t + 1 < NT:
                        load_x(t + 1)
                    s = 1 if t < NCT else 0
                    x, rpt = xs[t % 2], rp[t % 2]
                    B_ = sets[t % 2]
                    hf, hb, hT, pjs, t1, t2, t3, ss = B_["hf"], B_["hb"], B_["hT"], B_["pjs"], B_["t1"], B_["t2"], B_["t3"], B_["ss"]
                    stq, stk, vt, nrm, nT, mlq, mlkv, krr, junk = (B_["stq"], B_["stk"], B_["vt"], B_["nrm"], B_["nT"], B_["mlq"],
                                                                   B_["mlkv"], B_["krr"], B_["junk"])
                    tsl = slice(t * 128, (t + 1) * 128)
                    cos32, sin32, cos64, sin64 = rpt[:, 0:32], rpt[:, 32:64], rpt[:, 64:128], rpt[:, 128:192]
                    TT("dve", hf[:], x[:], A1[s][:], ALU.mult, [x, A1[s]], [hf])
                    TT("dve", hb[:], hf[:], B1[s][:], ALU.add, [hf, B1[s]], [hb])
                    pb = ptr[trans_store.n % 2]
                    trans_store.n += 1
                    for k in range(8):
                        TRB(pb[:, k, :], hb[:, k * 128:(k + 1) * 128], [hb], [pb])
                    CP("act", hT[:], pb[:], [pb], [hT])
                    P.dma("sp", hTs[t], hT[:], [hT], [hTs], "s1_hT%d" % (t % 2))
                    for nb in range(5):
                        c0, c1 = nb * 512, min(2464, nb * 512 + 512)
                        for k in range(8):
                            MM(pj[nb][:, 0:c1 - c0], hT[:, k, :], win[:, k, c0:c1], k == 0, k == 7, [hT, win], [pj[nb]])
                        CP("act", pjs[:, c0:c1], pj[nb][:, 0:c1 - c0], [pj[nb]], [pjs])
                    ACT(stq[:, 0:256], pjs[:, 0:256], AF.Copy, [pjs], [stq], scale=0.125)
                    CP("pool", stk[:, 0:256], pjs[:, 256:512], [pjs], [stk])
                    CP("pool", vt["na"][:, :, 0:64], pjs[:, 512:768].rearrange("p (h d) -> p h d", h=4), [pjs], [vt["na"]])
                    hv = lambda buf: buf.t.rearrange("(j hh) d t -> (hh d) j t", hh=2)
                    trans_store(stq, 256, [(hv(qT["na"])[:, j, tsl], qT["na"]) for j in range(2)], "naq")
                    trans_store(stk, 256, [(hv(kT["na"])[:, j, tsl], kT["na"]) for j in range(2)], "nak")
                    P.dma("sp", vS["na"][tsl], vt["na"][:], [vt["na"]], [vS["na"]], "s1_v0_%d" % (t % 2))
                    rope("dve", pjs[:, 768:1024].rearrange("p (g d) -> p g d", g=8), 8, 32, cos32, sin32, t1, t2,
                         stq[:, 0:256].rearrange("p (g d) -> p g d", g=8), [pjs], [stq], rpt)
                    rope("dve", pjs[:, 1024:1280].rearrange("p (g d) -> p g d", g=8), 8, 32, cos32, sin32, t1, t2,
                         stk[:, 0:256].rearrange("p (g d) -> p g d", g=8), [pjs], [stk], rpt)
                    CP("pool", vt["df"][:, :, 0:64], pjs[:, 1280:1536].rearrange("p (h d) -> p h d", h=4), [pjs], [vt["df"]])
                    trans_store(stq, 256, [(hv(qT["df"])[:, j, tsl], qT["df"]) for j in range(2)], "dfq")
                    trans_store(stk, 256, [(hv(kT["df"])[:, j, tsl], kT["df"]) for j in range(2)], "dfk")
                    P.dma("sp", vS["df"][tsl], vt["df"][:], [vt["df"]], [vS["df"]], "s1_v1_%d" % (t % 2))
                    for (c0, G, gain, stg) in ((1536, 4, gqn, stq), (1792, 2, gkn, stk)):
                        X = pjs[:, c0:c0 + G * 64]
                        X3 = X.rearrange("p (g d) -> p g d", g=G)
                        TT("dve", t3[:, 0:G * 64], X, X, ALU.mult, [pjs], [t3])
                        P.op("dve", lambda e, G=G, ss=ss, t3=t3: e.tensor_reduce(out=ss[:, 0:G], in_=t3[:, 0:G * 64].rearrange("p (g d) -> p g d", g=G),
                                                                     axis=AX.X, op=ALU.add), [t3], [ss])
                        rstd_from(ss, 1.0 / 64, G)
                        for g in range(G):
                            STT(t3[:, g * 64:(g + 1) * 64], X3[:, g, :], ss[:, g:g + 1], gain[:], ALU.mult, ALU.mult, [pjs, ss, gain], [t3])
                        rope("dve", t3[:, 0:G * 64].rearrange("p (g d) -> p g d", g=G), G, 64, cos64, sin64, t1, t2,
                             stg[:, 0:G * 64].rearrange("p (g d) -> p g d", g=G), [t3], [stg], rpt)
                    CP("pool", vt["gq"][:, :, 0:64], pjs[:, 1920:2048].rearrange("p (h d) -> p h d", h=2), [pjs], [vt["gq"]])
                    trans_store(stq, 256, [(hv(qT["gq"])[:, j, tsl], qT["gq"]) for j in range(2)], "gqq")
                    trans_store(stk, 128, [(kT["gq"].t.rearrange("g d t -> (g d) t")[:, tsl], kT["gq"])], "gqk")
                    P.dma("sp", vS["gq"][tsl], vt["gq"][:], [vt["gq"]], [vS["gq"]], "s1_v2_%d" % (t % 2))
                    ACT(junk[:, 0:256], pjs[:, 2048:2304], AF.Square, [pjs], [junk, ss], accum=ss[:, 0:1])
                    ACT(junk[:, 0:128], pjs[:, 2304:2432], AF.Square, [pjs], [junk, ss], accum=ss[:, 1:2])
                    ACT(ss[:, 0:1], ss[:, 0:1], AF.Sqrt, [ss], [ss], scale=1.0 / 256, bias=EPS)
                    ACT(ss[:, 1:2], ss[:, 1:2], AF.Sqrt, [ss], [ss], scale=1.0 / 128, bias=EPS)
                    P.op("dve", lambda e, ss=ss: e.reciprocal(out=ss[:, 0:2], in_=ss[:, 0:2]), [ss], [ss])
                    STT(nrm[:, 0:256], pjs[:, 2048:2304], ss[:, 0:1], qan[:], ALU.mult, ALU.mult, [pjs, ss, qan], [nrm])
                    pb = ptr[trans_store.n % 2]
                    trans_store.n += 1
                    for c in range(2):
                        TRB(pb[:, c, :], nrm[:, c * 128:(c + 1) * 128], [nrm], [pb])
                    CP("act", nT[:, 0:2, :], pb[:, 0:2, :], [pb], [nT])
                    for k in range(2):
                        MM(pj[0][:, 0:384], nT[:, k, :], wqb[:, k, :], k == 0, k == 1, [nT, wqb], [pj[0]])
                    CP("act", mlq[:], pj[0][:, 0:384], [pj[0]], [mlq])
                    STT(nrm[:, 0:128], pjs[:, 2304:2432], ss[:, 1:2], kvan[:], ALU.mult, ALU.mult, [pjs, ss, kvan], [nrm])
                    pb = ptr[trans_store.n % 2]
                    trans_store.n += 1
                    TRB(pb[:, 0, :], nrm[:, 0:128], [nrm], [pb])
                    CP("act", nT[:, 2, :], pb[:, 0, :], [pb], [nT])
                    MM(pj[1][:], nT[:, 2, :], wkvb[:], True, True, [nT, wkvb], [pj[1]])
                    CP("act", mlkv[:], pj[1][:], [pj[1]], [mlkv])
                    q3 = mlq[:].rearrange("p (h d) -> p h d", h=4)
                    sq3 = stq[:].rearrange("p (h d) -> p h d", h=4)
                    sk3 = stk[:].rearrange("p (h d) -> p h d", h=4)
                    kv3 = mlkv[:].rearrange("p (h d) -> p h d", h=4)
                    CP("pool", sq3[:, :, 0:64], q3[:, :, 0:64], [mlq], [stq])
                    rope("dve", q3[:, :, 64:96], 4, 32, cos32, sin32, t1, t2, sq3[:, :, 64:96], [mlq], [stq], rpt)
                    CP("pool", sk3[:, :, 0:64], kv3[:, :, 0:64], [mlkv], [stk])
                    rope("dve", pjs[:, 2432:2464].rearrange("p (g d) -> p g d", g=1), 1, 32, cos32, sin32, t1, t2,
                         krr[:].rearrange("p (g d) -> p g d", g=1), [pjs], [krr], rpt)
                    CP("dve", sk3[:, :, 64:96], krr[:].unsqueeze(1).broadcast_to([128, 4, 32]), [krr], [stk])
                    CP("pool", vt["ml"][:, :, 0:64], kv3[:, :, 64:128], [mlkv], [vt["ml"]])
                    fq = qT["ml"].t.rearrange("(c p) t -> p c t", p=128)
                    fk = kT["ml"].t.rearrange("(c p) t -> p c t", p=128)
                    trans_store(stq, 384, [(fq[:, c, tsl], qT["ml"]) for c in range(3)], "mlq")
                    trans_store(stk, 384, [(fk[:, c, tsl], kT["ml"]) for c in range(3)], "mlk")
                    P.dma("sp", vS["ml"][tsl], vt["ml"][:], [vt["ml"]], [vS["ml"]], "s1_v3_%d" % (t % 2))

        def stage2(l, last):
            lam_init = 0.8 - 0.6 * math.exp(-0.3 * l)
            qblocks = ([] if last else [(0, CTX, True)]) + [(CTX + i * 512, 512, False) for i in range(S // 512)]
            with P.scope():
                sel = P.sbuf("sel", [128, 128], F32)
                MEMSET("dve", sel[:], 0.0, [sel])
                CP("dve", sel[64:128, 0:64], idf[64:128, 64:128], [idf], [sel])
                ones64 = P.sbuf("ones64", [128, 128], F32)
                MEMSET("dve", ones64[:], 0.0, [ones64])
                MEMSET("dve", ones64[0:64, :], 1.0, [ones64])
                lamt = P.sbuf("lamt", [64, 128], F32)
                lam = P.sbuf("lam", [64, 4], F32)
                P.dma("sp", lamt[:], df_lam[l].partition_broadcast(64), [], [lamt], "s2_lam")
                junk = P.sbuf("s2_junk", [64, 32], F32)
                TT("dve", junk[:], lamt[:, 0:32], lamt[:, 32:64], ALU.mult, [lamt], [junk])
                P.op("dve", lambda e: e.tensor_reduce(out=lam[:, 0:1], in_=junk[:], axis=AX.X, op=ALU.add), [junk], [lam])
                TT("dve", junk[:], lamt[:, 64:96], lamt[:, 96:128], ALU.mult, [lamt], [junk])
                P.op("dve", lambda e: e.tensor_reduce(out=lam[:, 1:2], in_=junk[:], axis=AX.X, op=ALU.add), [junk], [lam])
                ACT(lam[:, 0:2], lam[:, 0:2], AF.Exp, [lam], [lam])
                TS("dve", lam[:, 2:3], lam[:, 1:2], lam[:, 0:1], ALU.subtract, [lam], [lam], s2=-lam_init, op1=ALU.add)
                subl = P.sbuf("subl", [64, 1], F32)
                P.dma("sp", subl[:], df_subln[l].rearrange("(d o) -> d o", o=1), [], [subl], "s2_subl")
                TS("dve", subl[:], subl[:], 1.0 - lam_init, ALU.mult, [subl], [subl])

                NST, NPT, NOP, NOSB, LA = 4, 4, 3, 4, 3
                ST = [P.psum("ST%d" % i, [128, 512], F32) for i in range(NST)]
                OP = [P.psum("OP%d" % i, [128, 512], F32) for i in range(NOP)]
                RB = [P.psum("RB%d" % i, [128, 512], F32) for i in range(1)]
                PT = [P.sbuf("PT%d" % i, [128, 512], BF16) for i in range(NPT)]
                osb = [P.sbuf("osb%d" % i, [128, 512], F32) for i in range(NOSB)]
                onb = [P.sbuf("onb%d" % i, [64, 512], BF16) for i in range(2)]
                cnt = {"st": 0, "op": 0, "fin": 0, "onb": 0}

                def fin_a(op_, n):
                    o = osb[cnt["fin"] % NOSB]
                    cnt["fin"] += 1
                    CP("act", o[:, 0:n], op_[:, 0:n], [op_], [o])
                    P.op("dve", lambda e: e.reciprocal(out=o[64:128, 0:n], in_=o[64:128, 0:n]), [o], [o])
                    return o

                def fin_b(o, n, want_f32=None):
                    rb = RB[0]
                    MM(rb[:, 0:n], sel[:], o[:, 0:n], True, True, [sel, o], [rb])
                    if want_f32 is not None:
                        TT("dve", want_f32[0:64, 0:n], o[0:64, 0:n], rb[0:64, 0:n], ALU.mult, [o, rb], [want_f32])
                        return None
                    ob = onb[cnt["onb"] % 2]
                    cnt["onb"] += 1
                    TT("dve", ob[:, 0:n], o[0:64, 0:n], rb[0:64, 0:n], ALU.mult, [o, rb], [ob])
                    return ob

                def run_pipeline(steps, DEFER=6):
                    n = len(steps)
                    sts = [(cnt["st"] + i) for i in range(n)]
                    cnt["st"] += n
                    pend = []
                    for i in range(n + LA):
                        if i < n:
                            steps[i]["qk"](ST[sts[i] % NST])
                        j = i - LA
                        while pend and pend[0][0] <= i:
                            pend.pop(0)[1]()
                        if j >= 0:
                            steps[j]["ex"](ST[sts[j] % NST], PT[sts[j] % NPT])
                            steps[j]["pv"](PT[sts[j] % NPT])
                            if steps[j].get("after") is not None:
                                r = steps[j]["after"]()
                                if r is not None:
                                    pend.append((i + DEFER, r))
                    for _, r in pend:
                        r()

                with P.scope():
                    KT = P.sbuf("na_KT", [128, 4, T], BF16)
                    V = P.sbuf("na_V", [128, NT, 4 * 128], BF16)
                    MEMSET("pool", KT[64:128, :, :], 0.0, [KT])
                    for h in range(4):
                        P.dma("sp", KT[0:64, h, :], kT["na"][h], [kT["na"]], [KT], "s2_kt")
                    vsrc = vS["na"].t.rearrange("(n p) h d -> p n (h d)", p=128)
                    for n0 in range(0, NT, 8):
                        n1 = min(NT, n0 + 8)
                        P.dma("sp", V[:, n0:n1, :], vsrc[:, n0:n1, :], [vS["na"]], [V], "s2_v")
                    btab = P.sbuf("na_bt", [128, NCFG * 4, 128], BF16)
                    bstg = [P.sbuf("na_bs%d" % i, [128, 4, 128], F32) for i in range(2)]
                    mstg = [P.sbuf("na_ms%d" % i, [128, 128], F32) for i in range(2)]
                    for cf in range(NCFG):
                        bs_, ms_ = bstg[cf % 2], mstg[cf % 2]
                        P.dma("sp", bs_[:], na_bias[l, cf].rearrange("h k q -> k h q"), [], [bs_], "s2_bs%d" % (cf % 2))
                        P.dma("sp", ms_[:], na_mask[cf], [], [ms_], "s2_ms%d" % (cf % 2))
                        TT("dve", btab[:, cf * 4:(cf + 1) * 4, :], bs_[:], ms_[:].unsqueeze(1).broadcast_to([128, 4, 128]), ALU.add,
                           [bs_, ms_], [btab])
                    QT = [P.sbuf("na_QT%d" % i, [128, 4, 256], BF16) for i in range(8)]
                    for q_ in QT:
                        MEMSET("pool", q_[64:128, :, :], 0.0, [q_])
                    nqi = 0
                    qtiles = ([] if last else [(0, CTX, None)]) + [(CTX + i * 128, 128, per_q[i]) for i in range(S // 128)]
                    steps = []
                    for (q0, nq, loc) in qtiles:
                        if nqi % 6 == 0 and steps:
                            run_pipeline(steps, DEFER=3)
                            steps = []
                        Q = QT[nqi % 8]
                        P.dma("sp", Q[0:64, :, 0:nq], qT["na"].t[:, :, q0:q0 + nq].rearrange("h d t -> d h t"), [qT["na"]], [Q], "s2_q%d" % (nqi % 8))
                        nqi += 1
                        for sub in range(nq // 128):
                            qs = slice(sub * 128, (sub + 1) * 128)
                            keys = [(kt, None) for kt in range(NCT)]
                            if loc is not None:
                                keys += [(kt + NCT, cf) for kt, cf in loc]
                            o_ps = OP[cnt["op"] % NOP]
                            cnt["op"] += 1
                            for ki, (kt, cf) in enumerate(keys):
                                def qk(st_, Q=Q, qs=qs, kt=kt, cf=cf):
                                    for h in range(4):
                                        MM(st_[:, h * 128:(h + 1) * 128], KT[:, h, kt * 128:(kt + 1) * 128], Q[:, h, qs], True, cf is None, [KT, Q], [st_])
                                        if cf is not None:
                                            MM(st_[:, h * 128:(h + 1) * 128], idb[:], btab[:, cf * 4 + h, :], False, True, [idb, btab], [st_])

                                def ex(st_, pt_):
                                    ACT(pt_[:], st_[:], AF.Exp, [st_], [pt_])

                                def pv(pt_, o_ps=o_ps, kt=kt, ki=ki, nk=len(keys)):
                                    for h in range(4):
                                        MM(o_ps[:, h * 128:(h + 1) * 128], V[:, kt, h * 128:(h + 1) * 128], pt_[:, h * 128:(h + 1) * 128],
                                           ki == 0, ki == nk - 1, [V, pt_], [o_ps])
                                after = None
                                if ki == len(keys) - 1:
                                    def after(o_ps=o_ps, q0=q0, sub=sub):
                                        o = fin_a(o_ps, 512)

                                        def part2():
                                            ob = fin_b(o, 512)
                                            P.dma("sp", oT[0][:, :, q0 + sub * 128:q0 + (sub + 1) * 128].rearrange("h d t -> d h t"),
                                                  ob[:].rearrange("d (h t) -> d h t", h=4), [ob], [oT], "s2_o%d" % ((cnt["onb"] - 1) % 2))
                                        return part2
                                steps.append(dict(qk=qk, ex=ex, pv=pv, after=after))
                    if steps:
                        run_pipeline(steps, DEFER=3)

                def dense(bi, name, nkv, nqh, dk, scale, maps):
                    with P.scope():
                        KT = P.sbuf(name + "_KT", [128, nkv, T], BF16)
                        V = P.sbuf(name + "_V", [128, NT, nkv * 128], BF16)
                        MEMSET("pool", KT[64:128, :, :], 0.0, [KT])
                        if name == "ml":
                            for h in range(4):
                                P.dma("sp", KT[0:96, h, :], kT["ml"][h * 96:(h + 1) * 96, :], [kT["ml"]], [KT], "s2_kt")
                        else:
                            for h in range(nkv):
                                P.dma("sp", KT[0:64, h, :], kT[name][h], [kT[name]], [KT], "s2_kt")
                        vsrc = vS[name].t.rearrange("(n p) h d -> p n (h d)", p=128)
                        for n0 in range(0, NT, 8):
                            n1 = min(NT, n0 + 8)
                            P.dma("sp", V[:, n0:n1, :], vsrc[:, n0:n1, :], [vS[name]], [V], "s2_v")
                        nmap = 2 if name == "df" else 1
                        QT = [[P.sbuf("%s_QT%d_%d" % (name, i, m), [128, nqh, 512], BF16) for m in range(nmap)] for i in range(2)]
                        for i in range(2):
                            for m in range(nmap):
                                MEMSET("dve", QT[i][m][:], 0.0, [QT[i][m]])
                        if name == "df":
                            d1 = [P.sbuf("df_d1%d" % i, [64, 512], F32) for i in range(2)]
                            d2 = [P.sbuf("df_d2%d" % i, [128, 512], F32) for i in range(2)]
                            for b_ in d2:
                                MEMSET("dve", b_[:], 0.0, [b_])
                            d3 = P.sbuf("df_d3", [64, 512], F32)
                            dob = [P.sbuf("df_ob%d" % i, [64, 512], BF16) for i in range(2)]

                        def load_q(bix):
                            q0, nq, isctx = qblocks[bix]
                            Q = QT[bix % 2]
                            if name == "df":
                                src = qT["df"].t[:, :, q0:q0 + nq].rearrange("h d t -> d h t")
                                P.dma("sp", Q[0][0:32, :, 0:nq], src[0:32], [qT["df"]], [Q[0]], "s2_q%d" % (bix % 2))
                                P.dma("sp", Q[1][32:64, :, 0:nq], src[32:64], [qT["df"]], [Q[1]], "s2_qb%d" % (bix % 2))
                            elif name == "ml":
                                for h in range(4):
                                    P.dma("sp", Q[0][0:96, h, 0:nq], qT["ml"][h * 96:(h + 1) * 96, q0:q0 + nq], [qT["ml"]], [Q[0]], "s2_q%d" % (bix % 2))
                            else:
                                P.dma("sp", Q[0][0:64, :, 0:nq], qT[name].t[:, :, q0:q0 + nq].rearrange("h d t -> d h t"), [qT[name]], [Q[0]], "s2_q%d" % (bix % 2))

                        load_q(0)
                        for bix, (q0, nq, isctx) in enumerate(qblocks):
                            if bix + 1 < len(qblocks):
                                load_q(bix + 1)
                            Q = QT[bix % 2]
                            kts = list(range(NCT)) if isctx else list(range(NT))
                            steps = []
                            for h in range(nqh):
                                g = h * nkv // nqh
                                ops_ = []
                                for m in range(nmap):
                                    o_ps = OP[cnt["op"] % NOP]
                                    cnt["op"] += 1
                                    ops_.append(o_ps)
                                    for j, kt in enumerate(kts):
                                        def qk(st_, g=g, kt=kt, m=m, h=h):
                                            MM(st_[:, 0:nq], KT[:, g, kt * 128:(kt + 1) * 128], Q[m][:, h, 0:nq], True, True, [KT, Q[m]], [st_])

                                        def ex(st_, pt_):
                                            ACT(pt_[:, 0:nq], st_[:, 0:nq], AF.Exp, [st_], [pt_], scale=scale)

                                        def pv(pt_, o_ps=o_ps, g=g, kt=kt, j=j):
                                            MM(o_ps[:, 0:nq], V[:, kt, g * 128:(g + 1) * 128], pt_[:, 0:nq], j == 0, j == len(kts) - 1, [V, pt_], [o_ps])
                                        after = None
                                        if j == len(kts) - 1 and name == "df":
                                            if m == 0:
                                                def after(o_ps=o_ps, h=h):
                                                    stash[h] = fin_a(o_ps, nq)
                                                    return None
                                            else:
                                                def after(o_ps=o_ps, h=h):
                                                    oa = stash[h]
                                                    obb = fin_a(o_ps, nq)

                                                    def part2():
                                                        a1, a2 = d1[h % 2], d2[h % 2]
                                                        fin_b(oa, nq, want_f32=a1)
                                                        fin_b(obb, nq, want_f32=a2)
                                                        STT(a1[:, 0:nq], a2[0:64, 0:nq], lam[:, 2:3], a1[:, 0:nq], ALU.mult, ALU.add, [a1, a2, lam], [a1])
                                                        TT("dve", a2[0:64, 0:nq], a1[:, 0:nq], a1[:, 0:nq], ALU.mult, [a1], [a2])
                                                        rb = RB[0]
                                                        MM(rb[:, 0:nq], ones64[:], a2[:, 0:nq], True, True, [ones64, a2], [rb])
                                                        ACT(d3[:, 0:nq], rb[0:64, 0:nq], AF.Sqrt, [rb], [d3], scale=1.0 / 64, bias=EPS)
                                                        P.op("dve", lambda e, nq=nq: e.reciprocal(out=d3[:, 0:nq], in_=d3[:, 0:nq]), [d3], [d3])
                                                        ob = dob[h % 2]
                                                        STT(ob[:, 0:nq], a1[:, 0:nq], subl[:, 0:1], d3[:, 0:nq], ALU.mult, ALU.mult, [a1, subl, d3], [ob])
                                                        P.dma("sp", oT[bi, h][:, q0:q0 + nq], ob[:, 0:nq], [ob], [oT], "s2_od%d" % (h % 2))
                                                    return part2
                                        elif j == len(kts) - 1:
                                            def after(o_ps=o_ps, h=h):
                                                o = fin_a(o_ps, nq)

                                                def part2():
                                                    ob = fin_b(o, nq)
                                                    P.dma("sp", oT[bi, h][:, q0:q0 + nq], ob[:, 0:nq], [ob], [oT], "s2_o%d" % ((cnt["onb"] - 1) % 2))
                                                return part2
                                        steps.append(dict(qk=qk, ex=ex, pv=pv, after=after))
                            stash = {}
                            run_pipeline(steps, DEFER=6)

                dense(1, "df", 4, 4, 64, 32 ** -0.5, 2)
                dense(2, "gq", 2, 4, 64, 64 ** -0.5, 1)
                dense(3, "ml", 4, 4, 96, 96 ** -0.5, 1)

        def stage3(l, last):
            blocks = ([] if last else [(0, CTX, 1)]) + [(CTX + i * 512, 512, 0) for i in range(S // 512)]
            with P.scope():
                wg = P.sbuf("wg", [128, 8, 4096], BF16)
                wsrc = w_in[l][:, 2464:6560].rearrange("(k p) n -> p k n", p=128)
                for k in range(8):
                    P.dma("pool", wg[:, k, :], wsrc[:, k, :], [], [wg], "s3_wg")
                wb = P.sbuf("wb", [128, 8, D], BF16)
                P.dma("pool", wb[:], w_branch[l].rearrange("(j p) n -> p j n", p=128), [], [wb], "s3_wb")
                wo = P.sbuf("wo", [128, 8, D], BF16)
                P.dma("pool", wo[:], w_out[l].rearrange("(k p) n -> p k n", p=128), [], [wo], "s3_wo")
                rw = P.sbuf("rw", [128, 8, NE], F32)
                P.dma("sp", rw[:], router_w[l].rearrange("(k p) n -> p k n", p=128), [], [rw], "s3_rw")
                rbias = P.sbuf("rbias", [128, NE], F32)
                P.dma("sp", rbias[:], router_b[l].partition_broadcast(128), [], [rbias], "s3_rb")
                G1 = P.sbuf("G1", [128, D], F32)
                A2 = P.sbuf("A2", [128, D], F32)
                B2 = P.sbuf("B2", [128, D], F32)
                lg = P.sbuf("lg", [128, D], F32)
                lb = P.sbuf("lb", [128, D], F32)
                P.dma("sp", lg[:], ln1_g[l].partition_broadcast(128), [], [lg], "s3_lg")
                P.dma("sp", lb[:], ln1_b[l].partition_broadcast(128), [], [lb], "s3_lb")
                hTb = P.sbuf("hTb", [128, 8, 512], BF16)
                ot = [P.sbuf("ot%d" % i, [128, 2, 512], BF16) for i in range(4)]
                yT = P.sbuf("yT", [128, 8, 512], BF16)
                sig = [P.sbuf("sig%d" % i, [128, 512], F32) for i in range(2)]
                term = P.sbuf("term", [128, 512], F32)
                yacc = P.sbuf("yacc", [128, 512], F32)
                xs = P.sbuf("s3_xs", [128, D], F32)
                rr = P.sbuf("s3_r", [128, D], F32)
                tmp = P.sbuf("s3_tmp", [128, D], F32)
                x1 = P.sbuf("s3_x1", [128, D], F32)
                h2 = P.sbuf("s3_h2", [128, D], F32)
                h2T = P.sbuf("s3_h2T", [128, 8, 128], F32)
                h2b = P.sbuf("s3_h2b", [128, D], BF16)
                tri = P.sbuf("s3_tri", [128, 128], F32)
                onesm = P.sbuf("s3_onesm", [128, 128], F32)
                P.dma("sp", tri[:], tri_in, [], [tri], "s3_tri")
                MEMSET("dve", onesm[:], 1.0, [onesm])
                cnt = P.sbuf("s3_cnt", [128, NE], F32)
                MEMSET("dve", cnt[:], 0.0, [cnt])
                rnk = P.sbuf("s3_rnk", [128, NE], F32)
                st6 = P.sbuf("s3_st6", [128, 2, 6], F32)
                mv = P.sbuf("s3_mv", [128, 2], F32)
                lgt = P.sbuf("s3_lgt", [128, NE], F32)
                m8 = P.sbuf("s3_m8", [128, 8], F32)
                msk = P.sbuf("s3_msk", [128, NE], F32)
                sm = P.sbuf("s3_sm", [128, 2], F32)
                comb = P.sbuf("s3_comb", [128, 128], F32)
                MEMSET("dve", comb[:], 0.0, [comb])
                pG = [P.psum("pG%d" % i, [128, 512], F32) for i in range(2)]
                pB = [P.psum("pB%d" % i, [128, 512], F32) for i in range(2)]
                pO = [P.psum("pO%d" % i, [128, 512], F32) for i in range(2)]
                pT_ = [P.psum("pT%d" % i, [128, 4, 128], F32) for i in range(2)]
                cur_s = None
                n = 0
                for (t0, nt, s) in blocks:
                    if s != cur_s:
                        cur_s = s
                        P.dma("sp", G1[:], MOD[s, :, 2048:3072], [MOD], [G1], "s3_m0")
                        P.dma("sp", A2[:], MOD[s, :, 4096:5120], [MOD], [A2], "s3_m1")
                        P.dma("sp", B2[:], MOD[s, :, 3072:4096], [MOD], [B2], "s3_m2")
                    ntile = nt // 128
                    for ti in range(ntile):
                        P.dma("sp", hTb[:, :, ti * 128:(ti + 1) * 128], hTs[t0 // 128 + ti], [hTs], [hTb], "s3_hT")
                    for i in range(4):
                        P.dma("sp", ot[i][:, :, 0:nt], oT[i].rearrange("(j hh) d t -> (hh d) j t", hh=2)[:, :, t0:t0 + nt], [oT], [ot[i]], "s3_ot%d" % i)
                    for c in range(8):
                        for i in range(4):
                            g_, b_, sg = pG[n % 2], pB[n % 2], sig[n % 2]
                            n += 1
                            for k in range(8):
                                MM(g_[:, 0:nt], wg[:, k, i * 1024 + c * 128:i * 1024 + (c + 1) * 128], hTb[:, k, 0:nt], k == 0, k == 7, [wg, hTb], [g_])
                            ACT(sg[:, 0:nt], g_[:, 0:nt], AF.Sigmoid, [g_], [sg])
                            for j in range(2):
                                MM(b_[:, 0:nt], wb[:, i * 2 + j, c * 128:(c + 1) * 128], ot[i][:, j, 0:nt], j == 0, j == 1, [wb, ot[i]], [b_])
                            if i == 0:
                                TT("dve", yacc[:, 0:nt], sg[:, 0:nt], b_[:, 0:nt], ALU.mult, [sg, b_], [yacc])
                            else:
                                TT("dve", term[:, 0:nt], sg[:, 0:nt], b_[:, 0:nt], ALU.mult, [sg, b_], [term])
                                if i < 3:
                                    TT("pool", yacc[:, 0:nt], yacc[:, 0:nt], term[:, 0:nt], ALU.add, [yacc, term], [yacc])
                                else:
                                    TT("pool", yT[:, c, 0:nt], yacc[:, 0:nt], term[:, 0:nt], ALU.add, [yacc, term], [yT])
                    for ti in range(ntile):
                        tg = t0 // 128 + ti
                        rows = slice(tg * 128, (tg + 1) * 128)
                        if l == 0:
                            src = ctx_in[rows, :] if tg < NCT else x_in[(tg - NCT) * 128:(tg - NCT + 1) * 128, :]
                            P.dma("sp", xs[:], src, [], [xs], "s3_x")
                        else:
                            P.dma("sp", xs[:], xB[rows, :], [xB], [xs], "s3_x")
                        for hf_ in range(2):
                            for c in range(8):
                                MM(pO[hf_][:], yT[:, c, ti * 128:(ti + 1) * 128], wo[:, c, hf_ * 512:(hf_ + 1) * 512], c == 0, c == 7, [yT, wo], [pO[hf_]])
                            TT("dve", tmp[:, hf_ * 512:(hf_ + 1) * 512], pO[hf_][:], G1[:, hf_ * 512:(hf_ + 1) * 512], ALU.mult, [pO[hf_], G1], [tmp])
                        STT(rr[:], xs[:], ALPHA, tmp[:], ALU.mult, ALU.add, [xs, tmp], [rr])
                        layer_norm(rr, tmp, lg, lb, st6, mv, x1)
                        P.dma("sp", xA[rows, :], x1[:], [x1], [xA], "s3_xA")
                        TT("pool", tmp[:], x1[:], A2[:], ALU.mult, [x1, A2], [tmp])
                        TT("dve", h2[:], tmp[:], B2[:], ALU.add, [tmp, B2], [h2])
                        for hf_ in range(2):
                            for k in range(4):
                                kk = hf_ * 4 + k
                                TRF(pT_[hf_][:, k, :], h2[:, kk * 128:(kk + 1) * 128], [h2], [pT_[hf_]])
                            CP("act", h2T[:, hf_ * 4:(hf_ + 1) * 4, :], pT_[hf_][:], [pT_[hf_]], [h2T])
                        CP("pool", h2b[:], h2[:], [h2], [h2b])
                        P.dma("sp", h2bS[rows, :], h2b[:], [h2b], [h2bS], "s3_h2b")
                        pl = pB[0]
                        for k in range(8):
                            MM(pl[:, 0:NE], h2T[:, k, :], rw[:, k, :], k == 0, k == 7, [h2T, rw], [pl])
                        TT("dve", lgt[:], pl[:, 0:NE], rbias[:], ALU.add, [pl, rbias], [lgt])
                        P.op("dve", lambda e: e.max(out=m8[:], in_=lgt[:]), [lgt], [m8])
                        TS("dve", msk[:], lgt[:], m8[:, 3:4], ALU.is_ge, [lgt, m8], [msk])
                        TS("dve", sm[:, 0:1], m8[:, 0:1], -1.0, ALU.mult, [m8], [sm])
                        ACT(lgt[:], lgt[:], AF.Exp, [lgt, sm], [lgt], bias=sm[:, 0:1])
                        TT("dve", lgt[:], lgt[:], msk[:], ALU.mult, [lgt, msk], [lgt])
                        P.op("dve", lambda e: e.tensor_reduce(out=sm[:, 1:2], in_=lgt[:], axis=AX.X, op=ALU.add), [lgt], [sm])
                        P.op("dve", lambda e: e.reciprocal(out=sm[:, 1:2], in_=sm[:, 1:2]), [sm], [sm])
                        TS("dve", comb[:, 0:NE], lgt[:], sm[:, 1:2], ALU.mult, [lgt, sm], [comb])
                        P.dma("sp", combS[rows, :], comb[:], [comb], [combS], "s3_cb")
                        pr = pB[1]
                        MM(pr[:, 0:NE], tri[:], msk[:], True, True, [tri, msk], [pr])
                        MM(pr[:, 64:64 + NE], onesm[:], msk[:], True, True, [onesm, msk], [pr])
                        TT("dve", rnk[:], pr[:, 0:NE], cnt[:], ALU.add, [pr, cnt], [rnk])
                        TT("dve", cnt[:], pr[:, 64:64 + NE], cnt[:], ALU.add, [pr, cnt], [cnt])
                        P.dma("sp", mskS[rows, :], msk[:], [msk], [mskS], "s3_mk")
                        P.dma("sp", rankS[rows, :], rnk[:], [rnk], [rankS], "s3_rk")

                P.dma("sp", cntS[:], cnt[:], [cnt], [cntS], "s3_cnt")

        def stage4(l, last):
            t_first = NCT if last else 0
            Tm = (NT - t_first) * 128
            nslot = (4 * Tm) // 512 + NE
            maxm = (Tm + 511) // 512
            w1rows = exp_w1.rearrange("l e d (g n) -> (l e d g) n", g=2)
            w2rows = exp_w2.rearrange("l e d n -> (l e d) n")
            b1rows = exp_b1.rearrange("l e (g n) -> (l e g) n", g=2)
            b2rows = exp_b2.rearrange("l e n -> (l e) n")
            with P.scope():
                posu = P.sbuf("posu", [128, NT, 4], U32)
                ck = P.sbuf("ck", [128, NT, 4], F32)
                idx = P.sbuf("idx", [128, 3, nslot, 8], U32)
                es = P.sbuf("a_es", [128, nslot], F32)
                bidx = P.sbuf("bidx", [128, 3, nslot], U32)
                with P.scope():
                    cnt = P.sbuf("a_cnt", [128, NE], F32)
                    P.dma("sp", cnt[:], cntS[:], [cntS], [cnt], "a_cnt")
                    pad = P.sbuf("a_pad", [128, NE], F32)
                    tmpe = P.sbuf("a_tmpe", [128, NE], F32)
                    TS("dve", pad[:], cnt[:], 0.0, ALU.is_gt, [cnt], [pad], s2=512.0, op1=ALU.mult)
                    for m in range(1, maxm):
                        TS("dve", tmpe[:], cnt[:], 512.0 * m, ALU.is_gt, [cnt], [tmpe], s2=512.0, op1=ALU.mult)
                        TT("dve", pad[:], pad[:], tmpe[:], ALU.add, [pad, tmpe], [pad])
                    start = P.sbuf("a_start", [128, NE], F32)
                    MEMSET("dve", start[:], 0.0, [start])
                    for e in range(1, NE):
                        TT("dve", start[:, e:e + 1], start[:, e - 1:e], pad[:, e - 1:e], ALU.add, [start, pad], [start])
                    endt = P.sbuf("a_end", [128, NE], F32)
                    TT("dve", endt[:], start[:], pad[:], ALU.add, [start, pad], [endt])
                    sst = P.sbuf("a_sst", [128, nslot], F32)
                    P.dma("sp", sst[:], sst_in[:, 0:nslot], [], [sst], "a_sst")
                    MEMSET("dve", es[:], 0.0, [es])
                    for e in range(NE):
                        STT(es[:], sst[:], endt[:, e:e + 1], es[:], ALU.is_ge, ALU.add, [sst, endt, es], [es])
                    TS("dve", es[:], es[:], float(NE - 1), ALU.min, [es], [es])
                    iok = P.sbuf("a_iok", [128, 8], F32)
                    P.dma("sp", iok[:], iok_in, [], [iok], "a_iok")
                    idf_ = P.sbuf("a_idf", [128, 3, nslot, 8], F32)
                    bdf_ = P.sbuf("a_bdf", [128, 3, nslot], F32)
                    for k in range(8):
                        TS("dve", idf_[:, 0, :, k], es[:], float(D), ALU.mult, [es, iok], [idf_], s2=iok[:, k:k + 1], op1=ALU.add)
                    TS("dve", idf_[:, 0, :, :], idf_[:, 0, :, :], float(l * NE * D), ALU.add, [idf_], [idf_])
                    TS("dve", idf_[:, 1, :, :], idf_[:, 0, :, :], 2.0, ALU.mult, [idf_], [idf_])
                    TS("dve", idf_[:, 2, :, :], idf_[:, 0, :, :], 2.0, ALU.mult, [idf_], [idf_], s2=1.0, op1=ALU.add)
                    CP("dve", idx[:], idf_[:], [idf_], [idx])
                    TS("dve", bdf_[:, 0, :], es[:], float(l * NE), ALU.add, [es], [bdf_])
                    TS("dve", bdf_[:, 1, :], bdf_[:, 0, :], 2.0, ALU.mult, [bdf_], [bdf_])
                    TS("dve", bdf_[:, 2, :], bdf_[:, 0, :], 2.0, ALU.mult, [bdf_], [bdf_], s2=1.0, op1=ALU.add)
                    CP("dve", bidx[:], bdf_[:], [bdf_], [bidx])
                    mk = [P.sbuf("a_mk%d" % i, [128, NE], F32) for i in range(2)]
                    rk = [P.sbuf("a_rk%d" % i, [128, NE], F32) for i in range(2)]
                    cb = [P.sbuf("a_cb%d" % i, [128, NE], F32) for i in range(2)]
                    hb_ = [P.sbuf("a_hb%d" % i, [128, D], BF16) for i in range(2)]
                    p1 = P.sbuf("a_p1", [128, NE], F32)
                    m8 = P.sbuf("a_m8", [128, 8], F32)
                    pf = P.sbuf("a_pf", [128, 4], F32)
                    for t in range(t_first, NT):
                        i = t % 2
                        rows = slice(t * 128, (t + 1) * 128)
                        P.dma("sp", mk[i][:], mskS[rows, :], [mskS], [mk[i]], "a_l0%d" % i)
                        P.dma("sp", rk[i][:], rankS[rows, :], [rankS], [rk[i]], "a_l1%d" % i)
                        P.dma("sp", cb[i][:], combS[rows, 0:NE], [combS], [cb[i]], "a_l2%d" % i)
                        P.dma("sp", hb_[i][:], h2bS[rows, :], [h2bS], [hb_[i]], "a_l3%d" % i)
                        TT("dve", p1[:], rk[i][:], start[:], ALU.add, [rk[i], start], [p1])
                        STT(p1[:], p1[:], 1.0, mk[i][:], ALU.add, ALU.mult, [p1, mk[i]], [p1])
                        P.op("dve", lambda e: e.max(out=m8[:], in_=p1[:]), [p1], [m8])
                        TS("dve", pf[:], m8[:, 0:4], -1.0, ALU.add, [m8], [pf])
                        CP("dve", posu[:, t, :], pf[:], [pf], [posu])
                        for k in range(4):
                            STT(tmpe[:], p1[:], m8[:, k:k + 1], cb[i][:], ALU.is_equal, ALU.mult, [p1, m8, cb[i]], [tmpe])
                            P.op("dve", lambda e, k=k, t=t: e.tensor_reduce(out=ck[:, t, k:k + 1], in_=tmpe[:], axis=AX.X, op=ALU.add), [tmpe], [ck])
                        for k in range(4):
                            P._emit("pool", lambda e, k=k, t=t, i=i: e.indirect_dma_start(
                                out=XsT, out_offset=bass.IndirectOffsetOnAxis(ap=posu[:, t, k:k + 1], axis=0), in_=hb_[i][:], in_offset=None),
                                [hb_[i], posu], XsB[0:nslot], dma_key="a_sc", nowaw="a_sc")
                with P.scope():
                    NR = 5
                    ringt = [P.sbuf("ring%d" % i, [128, 8, D], BF16) for i in range(NR)]
                    ring = [[Buf("ring%d_%d" % (i, k), ringt[i].t[:, k, :]) for k in range(8)] for i in range(NR)]
                    bt = [[P.sbuf("bias%d_%d" % (i, g), [128, D], BF16) for g in range(1)] for i in range(2)]
                    b1r = P.sbuf("b1r", [128, 2 * D], F32)
                    MEMSET("dve", b1r[:], 0.0, [b1r])
                    P.dma("sp", b1r[0:NE, :], exp_b1[l], [], [b1r], "b_b1")
                    b1T = P.sbuf("b1T", [128, 16, NE], F32)
                    ioe = P.sbuf("ioe", [128, NE], F32)
                    P.dma("sp", ioe[:], ioe_in, [], [ioe], "b_ioe")
                    oh = [P.sbuf("oh%d" % i, [128, NE], F32) for i in range(2)]
                    b1x = [P.sbuf("b1x%d" % i, [128, 16, NE], F32) for i in range(2)]
                    b1s = [P.sbuf("b1s%d" % i, [128, 16], F32) for i in range(2)]
                    loads = []
                    for sl in range(nslot):
                        loads += [(sl, 1), (sl, 2), (sl, 0)]
                    issued = [0]

                    def ensure(n):
                        while issued[0] <= n and issued[0] < len(loads):
                            i = issued[0]
                            sl, part = loads[i]
                            rows_ap = w2rows if part == 0 else w1rows
                            for k in range(8):
                                b = ring[i % NR][k]
                                P._emit("pool", lambda e, b=b, sl=sl, part=part, k=k, rows_ap=rows_ap: e.indirect_dma_start(
                                    out=b[:], out_offset=None, in_=rows_ap,
                                    in_offset=bass.IndirectOffsetOnAxis(ap=idx[:, part, sl, k:k + 1], axis=0)),
                                    [idx], [b], dma_key="b_w%d_%d" % (i % NR, k))
                            if part == 1:
                                for g, rap in ((0, b2rows),):
                                    b = bt[sl % 2][g]
                                    P._emit("pool", lambda e, b=b, sl=sl, g=g, rap=rap: e.indirect_dma_start(
                                        out=b[:], out_offset=None, in_=rap,
                                        in_offset=bass.IndirectOffsetOnAxis(ap=bidx[:, g, sl:sl + 1], axis=0)),
                                        [bidx], [b], dma_key="b_b%d_%d" % (sl % 2, g))
                            issued[0] += 1

                    onerow = P.sbuf("onerow", [128, 512], BF16)
                    MEMSET("dve", onerow[:], 0.0, [onerow])
                    MEMSET("dve", onerow[0:1, :], 1.0, [onerow])
                    xts = [P.sbuf("b_xt%d" % i, [128, 4, D], BF16) for i in range(2)]
                    h2Ts_ = [P.sbuf("b_h2T%d" % i, [128, 8, 512], BF16) for i in range(2)]
                    actT = [P.sbuf("actT%d" % i, [128, 8, 512], BF16) for i in range(2)]
                    gt = [P.sbuf("gt%d" % i, [128, 512], F32) for i in range(2)]
                    sg = [P.sbuf("sg%d" % i, [128, 512], F32) for i in range(2)]
                    gs = [P.sbuf("gs%d" % i, [128, 512], F32) for i in range(2)]
                    up = [P.sbuf("up%d" % i, [128, 512], F32) for i in range(2)]
                    ysb = [P.sbuf("ysb%d" % i, [128, D], F32) for i in range(2)]
                    pq = [P.psum("b_pq%d" % i, [128, 512], F32) for i in range(4)]
                    py = [P.psum("b_py%d" % i, [128, 2, 512], F32) for i in range(1)]
                    ptr = [P.psum("b_ptr%d" % i, [128, 8, 128], BF16) for i in range(2)]
                    nq = [0, 0, 0]
                    for j in range(16):
                        pp = pq[j % 4]
                        TRF(pp[:, 0:128], b1r[:, j * 128:(j + 1) * 128], [b1r], [pp])
                        CP("act", b1T[:, j, :], pp[:, 0:NE], [pp], [b1T])
                    TS("dve", b1T[:, 8:16, :], b1T[:, 8:16, :], 1.0, ALU.add, [b1T], [b1T])

                    def load_x(sl):
                        xt = xts[sl % 2]
                        P.dma("sp", xt[:], XsB[sl].t.rearrange("(n p) d -> p n d", p=128), [XsB[sl]], [xt], "b_xt%d" % (sl % 2))

                    def trans_x(sl):
                        xt, h2T = xts[sl % 2], h2Ts_[sl % 2]
                        for ti in range(4):
                            pb = ptr[nq[2] % 2]
                            nq[2] += 1
                            for k in range(8):
                                TRB(pb[:, k, :], xt[:, ti, k * 128:(k + 1) * 128], [xt], [pb])
                            CP("act", h2T[:, :, ti * 128:(ti + 1) * 128], pb[:], [pb], [h2T])

                    for sl in range(nslot):
                        base = sl * 3
                        ensure(base + 4)
                        wgt, wup, w2 = ring[base % NR], ring[(base + 1) % NR], ring[(base + 2) % NR]
                        b2t = bt[sl % 2][0]
                        oh_, bx_, bs_ = oh[sl % 2], b1x[sl % 2], b1s[sl % 2]
                        TS("dve", oh_[:], ioe[:], es[:, sl:sl + 1], ALU.is_equal, [ioe, es], [oh_])
                        TT("dve", bx_[:], b1T[:], oh_[:].unsqueeze(1).broadcast_to([128, 16, NE]), ALU.mult, [b1T, oh_], [bx_])
                        P.op("dve", lambda e, bx_=bx_, bs_=bs_: e.tensor_reduce(out=bs_[:], in_=bx_[:], axis=AX.X, op=ALU.add), [bx_], [bs_])
                        if sl == 0:
                            load_x(0)
                            trans_x(0)
                        if sl + 1 < nslot:
                            load_x(sl + 1)
                        h2T = h2Ts_[sl % 2]
                        aT = actT[sl % 2]
                        for j in range(8):
                            pg_, pu_ = pq[nq[0] % 4], pq[(nq[0] + 1) % 4]
                            i2 = (nq[0] // 2) % 2
                            nq[0] += 2
                            for k in range(8):
                                MM(pg_[:], wgt[k][:, j * 128:(j + 1) * 128], h2T[:, k, :], k == 0, k == 7, [wgt[k], h2T], [pg_])
                            for k in range(8):
                                MM(pu_[:], wup[k][:, j * 128:(j + 1) * 128], h2T[:, k, :], k == 0, k == 7, [wup[k], h2T], [pu_])
                            TS("dve", gt[i2][:], pg_[:], bs_[:, j:j + 1], ALU.add, [pg_, bs_], [gt[i2]], s2=7.0, op1=ALU.min)
                            ACT(sg[i2][:], gt[i2][:], AF.Sigmoid, [gt[i2]], [sg[i2]], scale=1.702)
                            TT("dve", gs[i2][:], gt[i2][:], sg[i2][:], ALU.mult, [gt[i2], sg[i2]], [gs[i2]])
                            TS("dve", up[i2][:], pu_[:], bs_[:, 8 + j:9 + j], ALU.add, [pu_, bs_], [up[i2]], s2=-6.0, op1=ALU.max)
                            STT(aT[:, j, :], up[i2][:], 8.0, gs[i2][:], ALU.min, ALU.mult, [up[i2], gs[i2]], [aT])
                        ensure(base + 5)
                        if sl + 1 < nslot:
                            trans_x(sl + 1)
                        for ti in range(4):
                            yp = py[0]
                            ys_ = ysb[nq[1] % 2]
                            nq[1] += 1
                            for hf_ in range(2):
                                for j in range(8):
                                    MM(yp[:, hf_, :], aT[:, j, ti * 128:(ti + 1) * 128], w2[j][:, hf_ * 512:(hf_ + 1) * 512], j == 0, False, [aT, w2[j]], [yp])
                                MM(yp[:, hf_, :], onerow[:, 0:128], b2t[:, hf_ * 512:(hf_ + 1) * 512], False, True, [onerow, b2t], [yp])
                            CP("act", ys_[:], yp[:].rearrange("p a b -> p (a b)"), [yp], [ys_])
                            P.dma("sp", YsT[sl * 512 + ti * 128:sl * 512 + (ti + 1) * 128, :], ys_[:], [ys_], [YsB[sl]], "b_ys%d" % ((nq[1] - 1) % 2))
                with P.scope():
                    G2 = [P.sbuf("G2_%d" % s_, [128, D], F32) for s_ in range(1 if last else 2)]
                    for s_ in range(len(G2)):
                        P.dma("sp", G2[s_][:], MOD[s_, :, 5120:6144], [MOD], [G2[s_]], "c_g2%d" % s_)
                    lg = P.sbuf("c_lg", [128, D], F32)
                    lb = P.sbuf("c_lb", [128, D], F32)
                    P.dma("sp", lg[:], ln2_g[l].partition_broadcast(128), [], [lg], "c_lg")
                    P.dma("sp", lb[:], ln2_b[l].partition_broadcast(128), [], [lb], "c_lb")
                    gth = [[P.sbuf("c_g%d_%d" % (i, k), [128, D], F32) for k in range(4)] for i in range(2)]
                    xs = [P.sbuf("c_xs%d" % i, [128, D], F32) for i in range(2)]
                    acc = [P.sbuf("c_acc%d" % i, [128, D], F32) for i in range(2)]
                    st6 = P.sbuf("c_st6", [128, 2, 6], F32)
                    mv = P.sbuf("c_mv", [128, 2], F32)
                    for t in range(t_first, NT):
                        i = t % 2
                        rows = slice(t * 128, (t + 1) * 128)
                        s_ = 1 if t < NCT else 0
                        for k in range(4):
                            P._emit("pool", lambda e, k=k, t=t, i=i: e.indirect_dma_start(
                                out=gth[i][k][:], out_offset=None, in_=YsT,
                                in_offset=bass.IndirectOffsetOnAxis(ap=posu[:, t, k:k + 1], axis=0)),
                                YsB[0:nslot] + [posu], [gth[i][k]], dma_key="c_g%d_%d" % (i, k))
                        P.dma("sp", xs[i][:], xA[rows, :], [xA], [xs[i]], "c_x%d" % i)
                        a = acc[i]
                        TS("dve", a[:], gth[i][0][:], ck[:, t, 0:1], ALU.mult, [gth[i][0], ck], [a])
                        for k in range(1, 4):
                            STT(a[:], gth[i][k][:], ck[:, t, k:k + 1], a[:], ALU.mult, ALU.add, [gth[i][k], ck, a], [a])
                        TT("dve", a[:], a[:], G2[s_][:], ALU.mult, [a, G2[s_]], [a])
                        STT(a[:], xs[i][:], ALPHA, a[:], ALU.mult, ALU.add, [xs[i], a], [a])
                        layer_norm(a, xs[i], lg, lb, st6, mv, a, geng="dve")
                        if last:
                            P.dma("sp", out_ap[(t - NCT) * 128:(t - NCT + 1) * 128, :], a[:], [a], [], "c_out%d" % i)
                        else:
                            P.dma("sp", xB[rows, :], a[:], [a], [xB], "c_out%d" % i)

        for l in range(L):
            last = (l == L - 1)
            stage0(l)
            stage1(l, last)
            stage2(l, last)
            stage3(l, last)
            stage4(l, last)
        P.flush()
    return nc, NCFG


_CACHE = {}


def make_inputs(inputs, S, L, NE, n_cores):
    per_q, cfg_list = na_structure(S)
    ncfg = len(cfg_list)
    rpb = np.asarray(inputs["na_rpb"], np.float32)
    na_bias = np.stack([rpb[:, :, a, b] for (a, b, _) in cfg_list], axis=1).astype(np.float32)
    na_mask = np.stack([m for (_, _, m) in cfg_list], axis=0).astype(np.float32)
    shared = {
        "w_ada": inputs["w_ada"], "b_ada": inputs["b_ada"], "w_in": inputs["w_in"],
        "na_bias": np.ascontiguousarray(na_bias), "na_mask": na_mask,
        "df_lam": np.asarray(inputs["df_lam"]).reshape(L, 128), "df_subln": inputs["df_subln"],
        "gq_qnorm": inputs["gq_qnorm"], "gq_knorm": inputs["gq_knorm"],
        "ml_qa_norm": inputs["ml_qa_norm"], "ml_wq_b": inputs["ml_wq_b"],
        "ml_kva_norm": inputs["ml_kva_norm"], "ml_wkv_b": inputs["ml_wkv_b"],
        "w_branch": np.asarray(inputs["w_branch"]).reshape(L, 1024, D), "w_out": inputs["w_out"],
        "ln1_g": inputs["ln1_g"], "ln1_b": inputs["ln1_b"], "ln2_g": inputs["ln2_g"], "ln2_b": inputs["ln2_b"],
        "router_w": inputs["router_w"], "router_b": inputs["router_b"],
        "exp_w1": inputs["exp_w1"], "exp_b1": inputs["exp_b1"], "exp_w2": inputs["exp_w2"], "exp_b2": inputs["exp_b2"],
        "idn": np.eye(128, dtype=np.float32), "rope": rope_tables(S),
        "tri": np.triu(np.ones((128, 128), np.float32), 1),
        "slotstart": np.tile((512.0 * np.arange((4 * (CTX + S) + 511) // 512 + NE, dtype=np.float32))[None, :], (128, 1)),
        "iotak": (np.arange(8, dtype=np.float32)[None, :] * 128 + np.arange(128, dtype=np.float32)[:, None]),
        "iotae": np.tile(np.arange(NE, dtype=np.float32)[None, :], (128, 1)),
    }
    shared = {k: np.ascontiguousarray(np.asarray(v, np.float32)) for k, v in shared.items()}
    maps = []
    for b in range(n_cores):
        m = dict(shared)
        m["x"] = np.ascontiguousarray(np.asarray(inputs["x"][b], np.float32))
        m["ctx"] = np.ascontiguousarray(np.asarray(inputs["ctx"][b], np.float32))
        m["c2"] = np.ascontiguousarray(np.stack([np.asarray(inputs["c"][b], np.float32), np.asarray(inputs["c_ctx"], np.float32)], axis=0))
        maps.append(m)
    return maps


def kernel(**inputs):
    x = np.asarray(inputs["x"])
    B, S, _ = x.shape
    L = int(np.asarray(inputs["w_ada"]).shape[0])
    NE = int(np.asarray(inputs["router_w"]).shape[-1])
    key = (S, L, NE)
    if key not in _CACHE:
        _CACHE[key] = build(S, L, NE)[0]
    nc = _CACHE[key]
    maps = make_inputs(inputs, S, L, NE, B)
    res = run_bass_kernel_spmd(nc, maps, core_ids=list(range(B)))
    return np.stack([np.asarray(r["out"], np.float32) for r in res.results], axis=0)
```

```python
import contextlib
import math
import numpy as np
import concourse.bass as bass
import concourse.mybir as mybir
from concourse.bass_utils import run_bass_kernel_spmd

F32 = mybir.dt.float32
BF16 = mybir.dt.bfloat16
U32 = mybir.dt.uint32
AF = mybir.ActivationFunctionType
ALU = mybir.AluOpType
AX = mybir.AxisListType

D = 1024
CTX = 256
GRID_W = 64
NEXP = 32
EPS = 1e-6
NEG = -30000.0


class Buf:
    __slots__ = ("name", "last_w", "readers", "t")

    def __init__(self, name, t=None):
        self.name = name
        self.last_w = None
        self.readers = {}
        self.t = t

    def __getitem__(self, idx):
        return self.t[idx]


class Prog:
    ENGS = ("pe", "act", "dve", "pool", "sp")

    def __init__(self, nc, stack):
        self.nc = nc
        self.stack = stack
        self.streams = {e: [] for e in self.ENGS}
        self.count = {}
        self.clock = {e: {} for e in self.ENGS}
        self.ops = []
        self.sems = {}
        self.free_sems = []
        self.fence = 0
        self.root = stack
        for e in self.ENGS:
            self.sem(e)

    def sem(self, key):
        if key not in self.sems:
            if self.free_sems:
                self.sems[key], self.count[key] = self.free_sems.pop()
            else:
                self.nsem = getattr(self, "nsem", 0) + 1
                self.sems[key] = self.root.enter_context(self.nc.semaphore("s_%d" % self.nsem))
                self.count[key] = 0
        return self.sems[key]

    def _uniq(self, name):
        self.nalloc = getattr(self, "nalloc", 0) + 1
        return "%s_%d" % (name, self.nalloc)

    def sbuf(self, name, shape, dtype):
        return Buf(name, self.stack.enter_context(self.nc.sbuf_tensor(self._uniq(name), list(shape), dtype)))

    def psum(self, name, shape, dtype=F32):
        return Buf(name, self.stack.enter_context(self.nc.psum_tensor(self._uniq(name), list(shape), dtype)))

    def dram(self, name, shape, dtype):
        return Buf(name, self.nc.dram_tensor(name, list(shape), dtype, kind="Internal").ap())

    @contextlib.contextmanager
    def scope(self):
        old = self.stack
        with contextlib.ExitStack() as st:
            self.stack = st
            try:
                yield
            finally:
                self.flush()
                self.stack = old

    def _emit(self, eng, fn, reads, writes, dma_key=None, nowaw=None):
        deps = set()
        for b in reads:
            if b.last_w is not None:
                deps.add(b.last_w)
        for b in writes:
            if b.last_w is not None and not (nowaw is not None and self.ops[b.last_w][0] == nowaw):
                deps.add(b.last_w)
            deps.update(b.readers.values())
        clk = self.clock[eng]
        waits = {}
        for d in deps:
            if d < self.fence:
                continue
            dim, val, vc = self.ops[d]
            if eng == "pe" and dim == "pe" and dma_key is None:
                continue
            if clk.get(dim, 0) >= val:
                continue
            if waits.get(dim, 0) < val:
                waits[dim] = val
            for k, v in vc.items():
                if clk.get(k, 0) < v:
                    clk[k] = v
        if dma_key is not None:
            dim = dma_key
            self.sem(dim)
            self.count[dim] += 16
        else:
            dim = eng
            self.count[dim] += 1
        val = self.count[dim]
        vc = dict(clk)
        vc[dim] = val
        oid = len(self.ops)
        self.ops.append((dim, val, vc))
        ws = set(id(b) for b in writes)
        for b in writes:
            b.last_w = oid
            b.readers = {}
        for b in reads:
            if id(b) not in ws:
                b.readers[dim] = oid
        self.streams[eng].append(([(self.sems[k], v) for k, v in waits.items()], fn, self.sems[dim], dma_key is not None))
        return oid

    def op(self, eng, fn, reads=(), writes=()):
        return self._emit(eng, fn, reads, writes)

    def dma(self, q, out_ap, in_ap, reads, writes, key):
        return self._emit(q, lambda e: e.dma_start(out=out_ap, in_=in_ap), reads, writes, dma_key=key)

    def barrier(self):
        allv = dict((k, v) for k, v in self.count.items() if v > 0)
        for e in self.ENGS:
            clk = self.clock[e]
            waits = [(self.sems[k], v) for k, v in allv.items() if clk.get(k, 0) < v]
            for k, v in allv.items():
                clk[k] = max(clk.get(k, 0), v)
            if waits:
                self.streams[e].append((waits, None, None, False))
        self.fence = len(self.ops)

    def flush(self):
        nc = self.nc
        hand = {"pe": "tensor", "act": "scalar", "dve": "vector", "pool": "gpsimd", "sp": "sync"}
        self.barrier()
        if not any(self.streams[e] for e in self.ENGS):
            return
        streams = self.streams
        self.streams = {e: [] for e in self.ENGS}
        with nc.Block() as block:
            for e in self.ENGS:
                stream = streams[e]

                def body(eng, stream=stream):
                    for waits, fn, sem, is_dma in stream:
                        for s, v in waits:
                            eng.wait_ge(s, v)
                        if fn is not None:
                            fn(eng).then_inc(sem, 16 if is_dma else 1)

                getattr(block, hand[e])(body)
        for k in [k for k in self.sems if k not in self.ENGS]:
            self.free_sems.append((self.sems.pop(k), self.count.pop(k)))
            for e in self.ENGS:
                self.clock[e].pop(k, None)


def rope_tables(S):
    T = CTX + S
    tab = np.zeros((T, 192), np.float32)
    tab[:, 0:32] = 1.0
    tab[:, 64:128] = 1.0
    t = np.arange(S)
    pos = (t // GRID_W, t % GRID_W)
    for n, c0, s0 in ((32, 0, 32), (64, 64, 128)):
        m = n // 2
        q = m // 2
        inv = (10000.0 ** (-np.arange(0, m, 2, dtype=np.float32) / m)).astype(np.float32)
        for part in range(2):
            ang = pos[part].astype(np.float32)[:, None] * inv[None, :]
            cs, sn = np.cos(ang).astype(np.float32), np.sin(ang).astype(np.float32)
            o = part * m
            tab[CTX:, c0 + o:c0 + o + q] = cs
            tab[CTX:, c0 + o + q:c0 + o + 2 * q] = cs
            tab[CTX:, s0 + o:s0 + o + q] = -sn
            tab[CTX:, s0 + o + q:s0 + o + 2 * q] = sn
    return tab


def na_structure(S):
    rows = S // GRID_W
    kh, kw = min(8, rows), 16
    cfgs = {}
    cfg_list = []
    per_q = []
    c = np.arange(GRID_W)
    cs = np.clip(c - kw // 2, 0, GRID_W - kw)
    for qt in range(S // 128):
        r = np.array([2 * qt, 2 * qt + 1])
        rs = np.clip(r - kh // 2, 0, rows - kh)
        lo, hi = rs.min(), rs.max() + kh - 1
        lst = []
        for kt in range(lo // 2, hi // 2 + 1):
            key = (kt - qt, int(rs[0] - r[0]), int(rs[1] - r[1]))
            if key not in cfgs:
                krl, kc = np.divmod(np.arange(128), 64)
                rl, cc = np.divmod(np.arange(128), 64)
                kr = 2 * kt + krl[:, None]
                rr = 2 * qt + rl[None, :]
                rsq = rs[rl][None, :]
                valid = (kr >= rsq) & (kr < rsq + kh) & (kc[:, None] >= cs[cc][None, :]) & (kc[:, None] < cs[cc][None, :] + kw)
                a = np.clip(kr - rr + 7, 0, 14)
                b = np.clip(kc[:, None] - cc[None, :] + 15, 0, 30)
                cfgs[key] = len(cfg_list)
                cfg_list.append((a, b, np.where(valid, 0.0, NEG).astype(np.float32)))
            lst.append((kt, cfgs[key]))
        per_q.append(lst)
    return per_q, cfg_list


def build(S, L, NE=NEXP, dbg=False):
    T = CTX + S
    NT = T // 128
    NCT = CTX // 128
    per_q, cfg_list = na_structure(S)
    NCFG = len(cfg_list)
    ALPHA = (2 * L) ** 0.25

    nc = bass.Bass("TRN2", target_bir_lowering=False)

    def din(name, shape):
        return nc.dram_tensor(name, list(shape), F32, kind="ExternalInput").ap()

    x_in = din("x", [S, D])
    ctx_in = din("ctx", [CTX, D])
    c2_in = din("c2", [2, D])
    w_ada = din("w_ada", [L, D, 6 * D])
    b_ada = din("b_ada", [L, 6 * D])
    w_in = din("w_in", [L, D, 6560])
    na_bias = din("na_bias", [L, NCFG, 4, 128, 128])
    na_mask = din("na_mask", [NCFG, 128, 128])
    df_lam = din("df_lam", [L, 128])
    df_subln = din("df_subln", [L, 64])
    gq_qnorm = din("gq_qnorm", [L, 64])
    gq_knorm = din("gq_knorm", [L, 64])
    ml_qa_norm = din("ml_qa_norm", [L, 256])
    ml_wq_b = din("ml_wq_b", [L, 256, 384])
    ml_kva_norm = din("ml_kva_norm", [L, 128])
    ml_wkv_b = din("ml_wkv_b", [L, 128, 512])
    w_branch = din("w_branch", [L, 1024, D])
    w_out = din("w_out", [L, D, D])
    ln1_g = din("ln1_g", [L, D])
    ln1_b = din("ln1_b", [L, D])
    ln2_g = din("ln2_g", [L, D])
    ln2_b = din("ln2_b", [L, D])
    router_w = din("router_w", [L, D, NE])
    router_b = din("router_b", [L, NE])
    exp_w1 = din("exp_w1", [L, NE, D, 2 * D])
    exp_b1 = din("exp_b1", [L, NE, 2 * D])
    exp_w2 = din("exp_w2", [L, NE, D, D])
    exp_b2 = din("exp_b2", [L, NE, D])
    idn_in = din("idn", [128, 128])
    rope_in = din("rope", [T, 192])
    NSLOT = (4 * T + 511) // 512 + NE
    NPOS = NSLOT * 512
    tri_in = din("tri", [128, 128])
    sst_in = din("slotstart", [128, NSLOT])
    iok_in = din("iotak", [128, 8])
    ioe_in = din("iotae", [128, NE])
    out_ap = nc.dram_tensor("out", [S, D], F32, kind="ExternalOutput").ap()

    with contextlib.ExitStack() as root:
        P = Prog(nc, root)
        MOD = P.dram("MOD", [2, 128, 6 * D], F32)
        xA = P.dram("xA", [T, D], F32)
        xB = P.dram("xB", [T, D], F32)
        hTs = P.dram("hTs", [NT, 128, 8, 128], BF16)
        qT = {"na": P.dram("qT_na", [4, 64, T], BF16), "df": P.dram("qT_df", [4, 64, T], BF16),
              "gq": P.dram("qT_gq", [4, 64, T], BF16), "ml": P.dram("qT_ml", [384, T], BF16)}
        kT = {"na": P.dram("kT_na", [4, 64, T], BF16), "df": P.dram("kT_df", [4, 64, T], BF16),
              "gq": P.dram("kT_gq", [2, 64, T], BF16), "ml": P.dram("kT_ml", [384, T], BF16)}
        vS = {"na": P.dram("v_na", [T, 4, 128], BF16), "df": P.dram("v_df", [T, 4, 128], BF16),
              "gq": P.dram("v_gq", [T, 2, 128], BF16), "ml": P.dram("v_ml", [T, 4, 128], BF16)}
        oT = P.dram("oT", [4, 4, 64, T], BF16)
        h2bS = P.dram("h2bS", [T, D], BF16)
        mskS = P.dram("mskS", [T, NE], F32)
        rankS = P.dram("rankS", [T, NE], F32)
        cntS = P.dram("cntS", [128, NE], F32)
        XsT = nc.dram_tensor("Xs", [NPOS, D], BF16, kind="Internal").ap()
        YsT = nc.dram_tensor("Ys", [NPOS, D], F32, kind="Internal").ap()
        XsB = [Buf("Xs%d" % i, XsT[i * 512:(i + 1) * 512, :]) for i in range(NSLOT)]
        YsB = [Buf("Ys%d" % i, YsT[i * 512:(i + 1) * 512, :]) for i in range(NSLOT)]
        combS = P.dram("combS", [T, 128], F32)
        dbgbufs = {}

        idf = P.sbuf("idf", [128, 128], F32)
        idb = P.sbuf("idb", [128, 128], BF16)
        P.dma("sp", idf[:], idn_in, [], [idf], "c_idf")
        P.op("dve", lambda e: e.tensor_copy(out=idb[:], in_=idf[:]), [idf], [idb])

        with P.scope():
            zt = P.sbuf("zt", [128, 4, D], BF16)
            P.op("dve", lambda e: e.memset(zt[:], 0.0), [], [zt])
            for i in range(NSLOT):
                P.dma("sp", XsB[i].t.rearrange("(n p) d -> p n d", p=128), zt[:], [zt], [XsB[i]], "z_xs")

        def MM(out, lhsT, rhs, start, stop, R, W):
            P.op("pe", lambda e: e.matmul(out=out, lhsT=lhsT, rhs=rhs, start=start, stop=stop), R, W)

        def TRB(out, in_, R, W):
            P.op("pe", lambda e: e.transpose(out=out, in_=in_, identity=idb[:]), list(R) + [idb], W)

        def TRF(out, in_, R, W):
            P.op("pe", lambda e: e.transpose(out=out, in_=in_, identity=idf[:]), list(R) + [idf], W)

        def TT(eng, out, in0, in1, op, R, W):
            P.op(eng, lambda e: e.tensor_tensor(out=out, in0=in0, in1=in1, op=op), R, W)

        def TS(eng, out, in0, s1, op0, R, W, s2=None, op1=None):
            if op1 is None:
                P.op(eng, lambda e: e.tensor_scalar(out=out, in0=in0, scalar1=s1, scalar2=None, op0=op0), R, W)
            else:
                P.op(eng, lambda e: e.tensor_scalar(out=out, in0=in0, scalar1=s1, scalar2=s2, op0=op0, op1=op1), R, W)

        def STT(out, in0, sc, in1, op0, op1, R, W):
            P.op("dve", lambda e: e.scalar_tensor_tensor(out=out, in0=in0, scalar=sc, in1=in1, op0=op0, op1=op1), R, W)

        def ACT(out, in_, func, R, W, scale=1.0, bias=0.0, accum=None):
            if accum is None:
                P.op("act", lambda e: e.activation(out=out, in_=in_, func=func, bias=bias, scale=scale), R, W)
            else:
                P.op("act", lambda e: e.activation(out=out, in_=in_, func=func, bias=bias, scale=scale, accum_out=accum), R, W)

        def CP(eng, out, in_, R, W):
            if eng == "act":
                P.op("act", lambda e: e.copy(out=out, in_=in_), R, W)
            else:
                P.op(eng, lambda e: e.tensor_copy(out=out, in_=in_), R, W)

        def MEMSET(eng, ap, val, W):
            P.op(eng, lambda e: e.memset(ap, val), [], W)

        def rstd_from(ss, scale, G):
            ACT(ss[:, 0:G], ss[:, 0:G], AF.Sqrt, [ss], [ss], scale=scale, bias=EPS)
            P.op("dve", lambda e: e.reciprocal(out=ss[:, 0:G], in_=ss[:, 0:G]), [ss], [ss])

        def layer_norm(xt, tmp, gam, bet, st6, mv, outb, geng="pool"):
            P.op("dve", lambda e: e.bn_stats(out=st6[:, 0, :], in_=xt[:, 0:512]), [xt], [st6])
            P.op("dve", lambda e: e.bn_stats(out=st6[:, 1, :], in_=xt[:, 512:1024]), [xt], [st6])
            P.op("dve", lambda e: e.bn_aggr(out=mv[:, 0:2], in_=st6[:].rearrange("p a b -> p (a b)")), [st6], [mv])
            ACT(mv[:, 1:2], mv[:, 1:2], AF.Sqrt, [mv], [mv], scale=1.0, bias=EPS)
            P.op("dve", lambda e: e.reciprocal(out=mv[:, 1:2], in_=mv[:, 1:2]), [mv], [mv])
            TS("dve", tmp[:], xt[:], mv[:, 0:1], ALU.subtract, [xt, mv], [tmp], s2=mv[:, 1:2], op1=ALU.mult)
            TT(geng, tmp[:], tmp[:], gam[:], ALU.mult, [tmp, gam], [tmp])
            TT("dve", outb[:], tmp[:], bet[:], ALU.add, [tmp, bet], [outb])

        def stage0(l):
            with P.scope():
                c2s = P.sbuf("c2s", [128, D], F32)
                MEMSET("dve", c2s[:], 0.0, [c2s])
                P.dma("sp", c2s[0:2, :], c2_in, [], [c2s], "s0_c2")
                ACT(c2s[:], c2s[:], AF.Silu, [c2s], [c2s])
                pt = P.psum("s0_pt", [128, 8, 128], F32)
                cT = P.sbuf("cT", [128, 8, 128], F32)
                for k in range(8):
                    TRF(pt[:, k, :], c2s[:, k * 128:(k + 1) * 128], [c2s], [pt])
                CP("dve", cT[:], pt[:], [pt], [cT])
                ones = P.sbuf("s0_ones", [128, 128], F32)
                MEMSET("dve", ones[:], 1.0, [ones])
                rep = [P.sbuf("rep%d" % s, [128, 8, 128], BF16) for s in range(2)]
                for s in range(2):
                    for k in range(8):
                        TS("dve", rep[s][:, k, :], ones[:], cT[:, k, s:s + 1], ALU.mult, [ones, cT], [rep[s]])
                bb = P.sbuf("s0_bb", [128, 6 * D], F32)
                P.dma("sp", bb[:], b_ada[l].partition_broadcast(128), [], [bb], "s0_bb")
                wa = [P.sbuf("s0_wa%d" % i, [128, 8, 512], BF16) for i in range(2)]
                pm = [P.psum("s0_pm%d" % i, [128, 512], F32) for i in range(2)]
                mo = [P.sbuf("s0_mo%d" % i, [128, 512], F32) for i in range(2)]
                n = 0
                for j in range(12):
                    w = wa[j % 2]
                    P.dma("pool", w[:], w_ada[l][:, j * 512:(j + 1) * 512].rearrange("(k p) n -> p k n", p=128), [], [w], "s0_wa%d" % (j % 2))
                    for s in range(2):
                        pp, m = pm[n % 2], mo[n % 2]
                        n += 1
                        for k in range(8):
                            MM(pp[:], rep[s][:, k, :], w[:, k, :], k == 0, k == 7, [rep[s], w], [pp])
                        if j in (2, 3, 8, 9):
                            STT(m[:], pp[:], 1.0, bb[:, j * 512:(j + 1) * 512], ALU.add, ALU.add, [pp, bb], [m])
                        else:
                            TT("dve", m[:], pp[:], bb[:, j * 512:(j + 1) * 512], ALU.add, [pp, bb], [m])
                        P.dma("sp", MOD[s, :, j * 512:(j + 1) * 512], m[:], [m], [MOD], "s0_mo%d" % ((n - 1) % 2))

        def rope(eng, X, G, n, cos, sin, t1, t2, out, R, W, ropeb):
            q = n // 4
            v1 = t1[:, 0:G * n].rearrange("p (g d) -> p g d", g=G)
            v2 = t2[:, 0:G * n].rearrange("p (g d) -> p g d", g=G)
            TT(eng, v1, X, cos.unsqueeze(1).broadcast_to([128, G, n]), ALU.mult, list(R) + [ropeb], [t1])
            for part in range(2):
                for half in range(2):
                    o = part * 2 * q + half * q
                    so = part * 2 * q + (1 - half) * q
                    TT(eng, v2[:, :, o:o + q], X[:, :, so:so + q], sin[:, o:o + q].unsqueeze(1).broadcast_to([128, G, q]),
                       ALU.mult, list(R) + [ropeb], [t2])
            TT(eng, out, v1, v2, ALU.add, [t1, t2], W)

        def stage1(l, last):
            with P.scope():
                win = P.sbuf("win", [128, 8, 2464], BF16)
                wsrc = w_in[l][:, 0:2464].rearrange("(k p) n -> p k n", p=128)
                for k in range(0, 8, 2):
                    P.dma("pool", win[:, k:k + 2, :], wsrc[:, k:k + 2, :], [], [win], "s1_win")
                wqb = P.sbuf("wqb", [128, 2, 384], BF16)
                P.dma("pool", wqb[:], ml_wq_b[l].rearrange("(k p) n -> p k n", p=128), [], [wqb], "s1_wqb")
                wkvb = P.sbuf("wkvb", [128, 512], BF16)
                P.dma("pool", wkvb[:], ml_wkv_b[l], [], [wkvb], "s1_wkvb")
                gqn = P.sbuf("gqn", [128, 64], F32)
                gkn = P.sbuf("gkn", [128, 64], F32)
                qan = P.sbuf("qan", [128, 256], F32)
                kvan = P.sbuf("kvan", [128, 128], F32)
                P.dma("sp", gqn[:], gq_qnorm[l].partition_broadcast(128), [], [gqn], "s1_c0")
                P.dma("sp", gkn[:], gq_knorm[l].partition_broadcast(128), [], [gkn], "s1_c1")
                P.dma("sp", qan[:], ml_qa_norm[l].partition_broadcast(128), [], [qan], "s1_c2")
                P.dma("sp", kvan[:], ml_kva_norm[l].partition_broadcast(128), [], [kvan], "s1_c3")
                A1 = [P.sbuf("A1_%d" % s, [128, D], F32) for s in range(2)]
                B1 = [P.sbuf("B1_%d" % s, [128, D], F32) for s in range(2)]
                for s in range(2):
                    P.dma("sp", A1[s][:], MOD[s, :, 1024:2048], [MOD], [A1[s]], "s1_A%d" % s)
                    P.dma("sp", B1[s][:], MOD[s, :, 0:1024], [MOD], [B1[s]], "s1_B%d" % s)
                pj = [P.psum("pj%d" % i, [128, 512], F32) for i in range(5)]
                ptr = [P.psum("ptr%d" % i, [128, 8, 128], BF16) for i in range(2)]
                xs = [P.sbuf("s1_xs%d" % i, [128, D], F32) for i in range(3)]
                rp = [P.sbuf("s1_rp%d" % i, [128, 192], F32) for i in range(3)]
                def mkset(i):
                    d = {}
                    d["hf"] = P.sbuf("s1_hf%d" % i, [128, D], F32)
                    d["hb"] = P.sbuf("s1_hb%d" % i, [128, D], BF16)
                    d["hT"] = P.sbuf("s1_hT%d" % i, [128, 8, 128], BF16)
                    d["pjs"] = P.sbuf("pjs%d" % i, [128, 2464], F32)
                    d["t1"] = P.sbuf("s1_t1%d" % i, [128, 256], F32)
                    d["t2"] = P.sbuf("s1_t2%d" % i, [128, 256], F32)
                    d["t3"] = P.sbuf("s1_t3%d" % i, [128, 256], F32)
                    d["ss"] = P.sbuf("s1_ss%d" % i, [128, 8], F32)
                    d["stq"] = P.sbuf("stq%d" % i, [128, 384], BF16)
                    d["stk"] = P.sbuf("stk%d" % i, [128, 384], BF16)
                    d["vt"] = {"na": P.sbuf("vt_na%d" % i, [128, 4, 128], BF16), "df": P.sbuf("vt_df%d" % i, [128, 4, 128], BF16),
                               "gq": P.sbuf("vt_gq%d" % i, [128, 2, 128], BF16), "ml": P.sbuf("vt_ml%d" % i, [128, 4, 128], BF16)}
                    for b in d["vt"].values():
                        MEMSET("pool", b[:], 1.0, [b])
                    d["nrm"] = P.sbuf("s1_nrm%d" % i, [128, 256], BF16)
                    d["nT"] = P.sbuf("s1_nT%d" % i, [128, 3, 128], BF16)
                    d["mlq"] = P.sbuf("mlq%d" % i, [128, 384], F32)
                    d["mlkv"] = P.sbuf("mlkv%d" % i, [128, 512], F32)
                    d["krr"] = P.sbuf("krr%d" % i, [128, 32], F32)
                    d["junk"] = P.sbuf("s1_junk%d" % i, [128, 256], F32)
                    return d
                sets = [mkset(0), mkset(1)]
                tsbs = [P.sbuf("tsb%d" % i, [128, 8, 128], BF16) for i in range(3)]

                def load_x(t):
                    b = xs[t % 3]
                    if l == 0:
                        src = ctx_in[t * 128:(t + 1) * 128, :] if t < NCT else x_in[(t - NCT) * 128:(t - NCT + 1) * 128, :]
                        P.dma("sp", b[:], src, [], [b], "s1_x%d" % (t % 3))
                    else:
                        P.dma("sp", b[:], xB[t * 128:(t + 1) * 128, :], [xB], [b], "s1_x%d" % (t % 3))
                    P.dma("sp", rp[t % 3][:], rope_in[t * 128:(t + 1) * 128, :], [], [rp[t % 3]], "s1_r%d" % (t % 3))

                def trans_store(stg, ncol, dst_views, tag):
                    nch = ncol // 128
                    pb = ptr[trans_store.n % 2]
                    tsb = tsbs[trans_store.n % 3]
                    tkey = "s1_ts%d" % (trans_store.n % 3)
                    trans_store.n += 1
                    for c in range(nch):
                        TRB(pb[:, c, :], stg[:, c * 128:(c + 1) * 128], [stg], [pb])
                    CP("act", tsb[:, 0:nch, :], pb[:, 0:nch, :], [pb], [tsb])
                    for c in range(nch):
                        P.dma("sp", dst_views[c][0], tsb[:, c, :], [tsb], [dst_views[c][1]], tkey)
                trans_store.n = 0

                def phaseA(t):
                    if t + 1 < NT:
                        load_x(t + 1)
                    s = 1 if t < NCT else 0
                    x, rpt = xs[t % 3], rp[t % 3]
                    B_ = sets[t % 2]
                    hf, hb, hT, pjs, t1, t2, t3, ss = B_["hf"], B_["hb"], B_["hT"], B_["pjs"], B_["t1"], B_["t2"], B_["t3"], B_["ss"]
                    stq, stk, vt, nrm, nT, mlq, mlkv, krr, junk = (B_["stq"], B_["stk"], B_["vt"], B_["nrm"], B_["nT"], B_["mlq"],
                                                                   B_["mlkv"], B_["krr"], B_["junk"])
                    tsl = slice(t * 128, (t + 1) * 128)
                    cos32, sin32, cos64, sin64 = rpt[:, 0:32], rpt[:, 32:64], rpt[:, 64:128], rpt[:, 128:192]
                    TT("dve", hf[:], x[:], A1[s][:], ALU.mult, [x, A1[s]], [hf])
                    TT("dve", hb[:], hf[:], B1[s][:], ALU.add, [hf, B1[s]], [hb])
                    pb = ptr[trans_store.n % 2]
                    trans_store.n += 1
                    for k in range(8):
                        TRB(pb[:, k, :], hb[:, k * 128:(k + 1) * 128], [hb], [pb])
                    CP("act", hT[:], pb[:], [pb], [hT])
                    P.dma("sp", hTs[t], hT[:], [hT], [hTs], "s1_hT%d" % (t % 2))
                    for nb in range(5):
                        c0, c1 = nb * 512, min(2464, nb * 512 + 512)
                        for k in range(8):
                            MM(pj[nb][:, 0:c1 - c0], hT[:, k, :], win[:, k, c0:c1], k == 0, k == 7, [hT, win], [pj[nb]])
                        CP("act", pjs[:, c0:c1], pj[nb][:, 0:c1 - c0], [pj[nb]], [pjs])

                def phaseB(t):
                    s = 1 if t < NCT else 0
                    x, rpt = xs[t % 3], rp[t % 3]
                    B_ = sets[t % 2]
                    hf, hb, hT, pjs, t1, t2, t3, ss = B_["hf"], B_["hb"], B_["hT"], B_["pjs"], B_["t1"], B_["t2"], B_["t3"], B_["ss"]
                    stq, stk, vt, nrm, nT, mlq, mlkv, krr, junk = (B_["stq"], B_["stk"], B_["vt"], B_["nrm"], B_["nT"], B_["mlq"],
                                                                   B_["mlkv"], B_["krr"], B_["junk"])
                    tsl = slice(t * 128, (t + 1) * 128)
                    cos32, sin32, cos64, sin64 = rpt[:, 0:32], rpt[:, 32:64], rpt[:, 64:128], rpt[:, 128:192]
                    ACT(stq[:, 0:256], pjs[:, 0:256], AF.Copy, [pjs], [stq], scale=0.125)
                    CP("pool", stk[:, 0:256], pjs[:, 256:512], [pjs], [stk])
                    CP("pool", vt["na"][:, :, 0:64], pjs[:, 512:768].rearrange("p (h d) -> p h d", h=4), [pjs], [vt["na"]])
                    hv = lambda buf: buf.t.rearrange("(j hh) d t -> (hh d) j t", hh=2)
                    trans_store(stq, 256, [(hv(qT["na"])[:, j, tsl], qT["na"]) for j in range(2)], "naq")
                    trans_store(stk, 256, [(hv(kT["na"])[:, j, tsl], kT["na"]) for j in range(2)], "nak")
                    P.dma("sp", vS["na"][tsl], vt["na"][:], [vt["na"]], [vS["na"]], "s1_v0_%d" % (t % 2))
                    rope("dve", pjs[:, 768:1024].rearrange("p (g d) -> p g d", g=8), 8, 32, cos32, sin32, t1, t2,
                         stq[:, 0:256].rearrange("p (g d) -> p g d", g=8), [pjs], [stq], rpt)
                    rope("dve", pjs[:, 1024:1280].rearrange("p (g d) -> p g d", g=8), 8, 32, cos32, sin32, t1, t2,
                         stk[:, 0:256].rearrange("p (g d) -> p g d", g=8), [pjs], [stk], rpt)
                    CP("pool", vt["df"][:, :, 0:64], pjs[:, 1280:1536].rearrange("p (h d) -> p h d", h=4), [pjs], [vt["df"]])
                    trans_store(stq, 256, [(hv(qT["df"])[:, j, tsl], qT["df"]) for j in range(2)], "dfq")
                    trans_store(stk, 256, [(hv(kT["df"])[:, j, tsl], kT["df"]) for j in range(2)], "dfk")
                    P.dma("sp", vS["df"][tsl], vt["df"][:], [vt["df"]], [vS["df"]], "s1_v1_%d" % (t % 2))
                    for (c0, G, gain, stg) in ((1536, 4, gqn, stq), (1792, 2, gkn, stk)):
                        X = pjs[:, c0:c0 + G * 64]
                        X3 = X.rearrange("p (g d) -> p g d", g=G)
                        TT("dve", t3[:, 0:G * 64], X, X, ALU.mult, [pjs], [t3])
                        P.op("dve", lambda e, G=G, ss=ss, t3=t3: e.tensor_reduce(out=ss[:, 0:G], in_=t3[:, 0:G * 64].rearrange("p (g d) -> p g d", g=G),
                                                                     axis=AX.X, op=ALU.add), [t3], [ss])
                        rstd_from(ss, 1.0 / 64, G)
                        for g in range(G):
                            STT(t3[:, g * 64:(g + 1) * 64], X3[:, g, :], ss[:, g:g + 1], gain[:], ALU.mult, ALU.mult, [pjs, ss, gain], [t3])
                        rope("dve", t3[:, 0:G * 64].rearrange("p (g d) -> p g d", g=G), G, 64, cos64, sin64, t1, t2,
                             stg[:, 0:G * 64].rearrange("p (g d) -> p g d", g=G), [t3], [stg], rpt)
                    CP("pool", vt["gq"][:, :, 0:64], pjs[:, 1920:2048].rearrange("p (h d) -> p h d", h=2), [pjs], [vt["gq"]])
                    trans_store(stq, 256, [(hv(qT["gq"])[:, j, tsl], qT["gq"]) for j in range(2)], "gqq")
                    trans_store(stk, 128, [(kT["gq"].t.rearrange("g d t -> (g d) t")[:, tsl], kT["gq"])], "gqk")
                    P.dma("sp", vS["gq"][tsl], vt["gq"][:], [vt["gq"]], [vS["gq"]], "s1_v2_%d" % (t % 2))
                    ACT(junk[:, 0:256], pjs[:, 2048:2304], AF.Square, [pjs], [junk, ss], accum=ss[:, 0:1])
                    ACT(junk[:, 0:128], pjs[:, 2304:2432], AF.Square, [pjs], [junk, ss], accum=ss[:, 1:2])
                    ACT(ss[:, 0:1], ss[:, 0:1], AF.Sqrt, [ss], [ss], scale=1.0 / 256, bias=EPS)
                    ACT(ss[:, 1:2], ss[:, 1:2], AF.Sqrt, [ss], [ss], scale=1.0 / 128, bias=EPS)
                    P.op("dve", lambda e, ss=ss: e.reciprocal(out=ss[:, 0:2], in_=ss[:, 0:2]), [ss], [ss])
                    STT(nrm[:, 0:256], pjs[:, 2048:2304], ss[:, 0:1], qan[:], ALU.mult, ALU.mult, [pjs, ss, qan], [nrm])
                    pb = ptr[trans_store.n % 2]
                    trans_store.n += 1
                    for c in range(2):
                        TRB(pb[:, c, :], nrm[:, c * 128:(c + 1) * 128], [nrm], [pb])
                    CP("act", nT[:, 0:2, :], pb[:, 0:2, :], [pb], [nT])
                    for k in range(2):
                        MM(pj[0][:, 0:384], nT[:, k, :], wqb[:, k, :], k == 0, k == 1, [nT, wqb], [pj[0]])
                    CP("act", mlq[:], pj[0][:, 0:384], [pj[0]], [mlq])
                    STT(nrm[:, 0:128], pjs[:, 2304:2432], ss[:, 1:2], kvan[:], ALU.mult, ALU.mult, [pjs, ss, kvan], [nrm])
                    pb = ptr[trans_store.n % 2]
                    trans_store.n += 1
                    TRB(pb[:, 0, :], nrm[:, 0:128], [nrm], [pb])
                    CP("act", nT[:, 2, :], pb[:, 0, :], [pb], [nT])
                    MM(pj[1][:], nT[:, 2, :], wkvb[:], True, True, [nT, wkvb], [pj[1]])
                    CP("act", mlkv[:], pj[1][:], [pj[1]], [mlkv])
                    q3 = mlq[:].rearrange("p (h d) -> p h d", h=4)
                    sq3 = stq[:].rearrange("p (h d) -> p h d", h=4)
                    sk3 = stk[:].rearrange("p (h d) -> p h d", h=4)
                    kv3 = mlkv[:].rearrange("p (h d) -> p h d", h=4)
                    CP("pool", sq3[:, :, 0:64], q3[:, :, 0:64], [mlq], [stq])
                    rope("dve", q3[:, :, 64:96], 4, 32, cos32, sin32, t1, t2, sq3[:, :, 64:96], [mlq], [stq], rpt)
                    CP("pool", sk3[:, :, 0:64], kv3[:, :, 0:64], [mlkv], [stk])
                    rope("dve", pjs[:, 2432:2464].rearrange("p (g d) -> p g d", g=1), 1, 32, cos32, sin32, t1, t2,
                         krr[:].rearrange("p (g d) -> p g d", g=1), [pjs], [krr], rpt)
                    CP("dve", sk3[:, :, 64:96], krr[:].unsqueeze(1).broadcast_to([128, 4, 32]), [krr], [stk])
                    CP("pool", vt["ml"][:, :, 0:64], kv3[:, :, 64:128], [mlkv], [vt["ml"]])
                    fq = qT["ml"].t.rearrange("(c p) t -> p c t", p=128)
                    fk = kT["ml"].t.rearrange("(c p) t -> p c t", p=128)
                    trans_store(stq, 384, [(fq[:, c, tsl], qT["ml"]) for c in range(3)], "mlq")
                    trans_store(stk, 384, [(fk[:, c, tsl], kT["ml"]) for c in range(3)], "mlk")
                    P.dma("sp", vS["ml"][tsl], vt["ml"][:], [vt["ml"]], [vS["ml"]], "s1_v3_%d" % (t % 2))

                load_x(0)
                phaseA(0)
                for t in range(NT):
                    if t + 1 < NT:
                        phaseA(t + 1)
                    phaseB(t)

        def stage2(l, last):
            lam_init = 0.8 - 0.6 * math.exp(-0.3 * l)
            qblocks = ([] if last else [(0, CTX, True)]) + [(CTX + i * 512, 512, False) for i in range(S // 512)]
            with P.scope():
                sel = P.sbuf("sel", [128, 128], F32)
                MEMSET("dve", sel[:], 0.0, [sel])
                CP("dve", sel[64:128, 0:64], idf[64:128, 64:128], [idf], [sel])
                ones64 = P.sbuf("ones64", [128, 128], F32)
                MEMSET("dve", ones64[:], 0.0, [ones64])
                MEMSET("dve", ones64[0:64, :], 1.0, [ones64])
                lamt = P.sbuf("lamt", [64, 128], F32)
                lam = P.sbuf("lam", [64, 4], F32)
                P.dma("sp", lamt[:], df_lam[l].partition_broadcast(64), [], [lamt], "s2_lam")
                junk = P.sbuf("s2_junk", [64, 32], F32)
                TT("dve", junk[:], lamt[:, 0:32], lamt[:, 32:64], ALU.mult, [lamt], [junk])
                P.op("dve", lambda e: e.tensor_reduce(out=lam[:, 0:1], in_=junk[:], axis=AX.X, op=ALU.add), [junk], [lam])
                TT("dve", junk[:], lamt[:, 64:96], lamt[:, 96:128], ALU.mult, [lamt], [junk])
                P.op("dve", lambda e: e.tensor_reduce(out=lam[:, 1:2], in_=junk[:], axis=AX.X, op=ALU.add), [junk], [lam])
                ACT(lam[:, 0:2], lam[:, 0:2], AF.Exp, [lam], [lam])
                TS("dve", lam[:, 2:3], lam[:, 1:2], lam[:, 0:1], ALU.subtract, [lam], [lam], s2=-lam_init, op1=ALU.add)
                subl = P.sbuf("subl", [64, 1], F32)
                P.dma("sp", subl[:], df_subln[l].rearrange("(d o) -> d o", o=1), [], [subl], "s2_subl")
                TS("dve", subl[:], subl[:], 1.0 - lam_init, ALU.mult, [subl], [subl])

                NST, NPT, NOP, NOSB, LA = 4, 4, 3, 4, 3
                ST = [P.psum("ST%d" % i, [128, 512], F32) for i in range(NST)]
                OP = [P.psum("OP%d" % i, [128, 512], F32) for i in range(NOP)]
                RB = [P.psum("RB%d" % i, [128, 512], F32) for i in range(1)]
                PT = [P.sbuf("PT%d" % i, [128, 512], BF16) for i in range(NPT)]
                osb = [P.sbuf("osb%d" % i, [128, 512], F32) for i in range(NOSB)]
                onb = [P.sbuf("onb%d" % i, [64, 512], BF16) for i in range(2)]
                cnt = {"st": 0, "op": 0, "fin": 0, "onb": 0}

                def fin_a(op_, n):
                    o = osb[cnt["fin"] % NOSB]
                    cnt["fin"] += 1
                    CP("act", o[:, 0:n], op_[:, 0:n], [op_], [o])
                    P.op("dve", lambda e: e.reciprocal(out=o[64:128, 0:n], in_=o[64:128, 0:n]), [o], [o])
                    return o

                def fin_b(o, n, want_f32=None):
                    rb = RB[0]
                    MM(rb[:, 0:n], sel[:], o[:, 0:n], True, True, [sel, o], [rb])
                    if want_f32 is not None:
                        TT("dve", want_f32[0:64, 0:n], o[0:64, 0:n], rb[0:64, 0:n], ALU.mult, [o, rb], [want_f32])
                        return None
                    ob = onb[cnt["onb"] % 2]
                    cnt["onb"] += 1
                    TT("dve", ob[:, 0:n], o[0:64, 0:n], rb[0:64, 0:n], ALU.mult, [o, rb], [ob])
                    return ob

                def run_pipeline(steps, DEFER=6):
                    n = len(steps)
                    sts = [(cnt["st"] + i) for i in range(n)]
                    cnt["st"] += n
                    pend = []
                    for i in range(n + LA):
                        if i < n:
                            steps[i]["qk"](ST[sts[i] % NST])
                        j = i - LA
                        while pend and pend[0][0] <= i:
                            pend.pop(0)[1]()
                        if j >= 0:
                            steps[j]["ex"](ST[sts[j] % NST], PT[sts[j] % NPT])
                            steps[j]["pv"](PT[sts[j] % NPT])
                            if steps[j].get("after") is not None:
                                r = steps[j]["after"]()
                                if r is not None:
                                    pend.append((i + DEFER, r))
                    for _, r in pend:
                        r()

                with P.scope():
                    KT = P.sbuf("na_KT", [128, 4, T], BF16)
                    V = P.sbuf("na_V", [128, NT, 4 * 128], BF16)
                    MEMSET("pool", KT[64:128, :, :], 0.0, [KT])
                    for h in range(4):
                        P.dma("sp", KT[0:64, h, :], kT["na"][h], [kT["na"]], [KT], "s2_kt")
                    vsrc = vS["na"].t.rearrange("(n p) h d -> p n (h d)", p=128)
                    for n0 in range(0, NT, 8):
                        n1 = min(NT, n0 + 8)
                        P.dma("sp", V[:, n0:n1, :], vsrc[:, n0:n1, :], [vS["na"]], [V], "s2_v")
                    btab = P.sbuf("na_bt", [128, NCFG * 4, 128], BF16)
                    bstg = [P.sbuf("na_bs%d" % i, [128, 4, 128], F32) for i in range(2)]
                    mstg = [P.sbuf("na_ms%d" % i, [128, 128], F32) for i in range(2)]
                    for cf in range(NCFG):
                        bs_, ms_ = bstg[cf % 2], mstg[cf % 2]
                        P.dma("sp", bs_[:], na_bias[l, cf].rearrange("h k q -> k h q"), [], [bs_], "s2_bs%d" % (cf % 2))
                        P.dma("sp", ms_[:], na_mask[cf], [], [ms_], "s2_ms%d" % (cf % 2))
                        TT("dve", btab[:, cf * 4:(cf + 1) * 4, :], bs_[:], ms_[:].unsqueeze(1).broadcast_to([128, 4, 128]), ALU.add,
                           [bs_, ms_], [btab])
                    QT = [P.sbuf("na_QT%d" % i, [128, 4, 256], BF16) for i in range(8)]
                    for q_ in QT:
                        MEMSET("pool", q_[64:128, :, :], 0.0, [q_])
                    nqi = 0
                    qtiles = ([] if last else [(0, CTX, None)]) + [(CTX + i * 128, 128, per_q[i]) for i in range(S // 128)]
                    steps = []
                    for (q0, nq, loc) in qtiles:
                        if nqi % 6 == 0 and steps:
                            run_pipeline(steps, DEFER=3)
                            steps = []
                        Q = QT[nqi % 8]
                        P.dma("sp", Q[0:64, :, 0:nq], qT["na"].t[:, :, q0:q0 + nq].rearrange("h d t -> d h t"), [qT["na"]], [Q], "s2_q%d" % (nqi % 8))
                        nqi += 1
                        for sub in range(nq // 128):
                            qs = slice(sub * 128, (sub + 1) * 128)
                            keys = [(kt, None) for kt in range(NCT)]
                            if loc is not None:
                                keys += [(kt + NCT, cf) for kt, cf in loc]
                            o_ps = OP[cnt["op"] % NOP]
                            cnt["op"] += 1
                            for ki, (kt, cf) in enumerate(keys):
                                def qk(st_, Q=Q, qs=qs, kt=kt, cf=cf):
                                    for h in range(4):
                                        MM(st_[:, h * 128:(h + 1) * 128], KT[:, h, kt * 128:(kt + 1) * 128], Q[:, h, qs], True, cf is None, [KT, Q], [st_])
                                        if cf is not None:
                                            MM(st_[:, h * 128:(h + 1) * 128], idb[:], btab[:, cf * 4 + h, :], False, True, [idb, btab], [st_])

                                def ex(st_, pt_):
                                    ACT(pt_[:], st_[:], AF.Exp, [st_], [pt_])

                                def pv(pt_, o_ps=o_ps, kt=kt, ki=ki, nk=len(keys)):
                                    for h in range(4):
                                        MM(o_ps[:, h * 128:(h + 1) * 128], V[:, kt, h * 128:(h + 1) * 128], pt_[:, h * 128:(h + 1) * 128],
                                           ki == 0, ki == nk - 1, [V, pt_], [o_ps])
                                after = None
                                if ki == len(keys) - 1:
                                    def after(o_ps=o_ps, q0=q0, sub=sub):
                                        o = fin_a(o_ps, 512)

                                        def part2():
                                            ob = fin_b(o, 512)
                                            P.dma("sp", oT[0][:, :, q0 + sub * 128:q0 + (sub + 1) * 128].rearrange("h d t -> d h t"),
                                                  ob[:].rearrange("d (h t) -> d h t", h=4), [ob], [oT], "s2_o%d" % ((cnt["onb"] - 1) % 2))
                                        return part2
                                steps.append(dict(qk=qk, ex=ex, pv=pv, after=after))
                    if steps:
                        run_pipeline(steps, DEFER=3)

                def dense(bi, name, nkv, nqh, dk, scale, maps):
                    with P.scope():
                        KT = P.sbuf(name + "_KT", [128, nkv, T], BF16)
                        V = P.sbuf(name + "_V", [128, NT, nkv * 128], BF16)
                        MEMSET("pool", KT[64:128, :, :], 0.0, [KT])
                        if name == "ml":
                            for h in range(4):
                                P.dma("sp", KT[0:96, h, :], kT["ml"][h * 96:(h + 1) * 96, :], [kT["ml"]], [KT], "s2_kt")
                        else:
                            for h in range(nkv):
                                P.dma("sp", KT[0:64, h, :], kT[name][h], [kT[name]], [KT], "s2_kt")
                        vsrc = vS[name].t.rearrange("(n p) h d -> p n (h d)", p=128)
                        for n0 in range(0, NT, 8):
                            n1 = min(NT, n0 + 8)
                            P.dma("sp", V[:, n0:n1, :], vsrc[:, n0:n1, :], [vS[name]], [V], "s2_v")
                        nmap = 2 if name == "df" else 1
                        QT = [[P.sbuf("%s_QT%d_%d" % (name, i, m), [128, nqh, 512], BF16) for m in range(nmap)] for i in range(2)]
                        for i in range(2):
                            for m in range(nmap):
                                MEMSET("dve", QT[i][m][:], 0.0, [QT[i][m]])
                        if name == "df":
                            d1 = [P.sbuf("df_d1%d" % i, [64, 512], F32) for i in range(2)]
                            d2 = [P.sbuf("df_d2%d" % i, [128, 512], F32) for i in range(2)]
                            for b_ in d2:
                                MEMSET("dve", b_[:], 0.0, [b_])
                            d3 = P.sbuf("df_d3", [64, 512], F32)
                            dob = [P.sbuf("df_ob%d" % i, [64, 512], BF16) for i in range(2)]

                        def load_q(bix):
                            q0, nq, isctx = qblocks[bix]
                            Q = QT[bix % 2]
                            if name == "df":
                                src = qT["df"].t[:, :, q0:q0 + nq].rearrange("h d t -> d h t")
                                P.dma("sp", Q[0][0:32, :, 0:nq], src[0:32], [qT["df"]], [Q[0]], "s2_q%d" % (bix % 2))
                                P.dma("sp", Q[1][32:64, :, 0:nq], src[32:64], [qT["df"]], [Q[1]], "s2_qb%d" % (bix % 2))
                            elif name == "ml":
                                for h in range(4):
                                    P.dma("sp", Q[0][0:96, h, 0:nq], qT["ml"][h * 96:(h + 1) * 96, q0:q0 + nq], [qT["ml"]], [Q[0]], "s2_q%d" % (bix % 2))
                            else:
                                P.dma("sp", Q[0][0:64, :, 0:nq], qT[name].t[:, :, q0:q0 + nq].rearrange("h d t -> d h t"), [qT[name]], [Q[0]], "s2_q%d" % (bix % 2))

                        load_q(0)
                        for bix, (q0, nq, isctx) in enumerate(qblocks):
                            if bix + 1 < len(qblocks):
                                load_q(bix + 1)
                            Q = QT[bix % 2]
                            kts = list(range(NCT)) if isctx else list(range(NT))
                            steps = []
                            for h in range(nqh):
                                g = h * nkv // nqh
                                ops_ = []
                                for m in range(nmap):
                                    o_ps = OP[cnt["op"] % NOP]
                                    cnt["op"] += 1
                                    ops_.append(o_ps)
                                    for j, kt in enumerate(kts):
                                        def qk(st_, g=g, kt=kt, m=m, h=h):
                                            MM(st_[:, 0:nq], KT[:, g, kt * 128:(kt + 1) * 128], Q[m][:, h, 0:nq], True, True, [KT, Q[m]], [st_])

                                        def ex(st_, pt_):
                                            ACT(pt_[:, 0:nq], st_[:, 0:nq], AF.Exp, [st_], [pt_], scale=scale)

                                        def pv(pt_, o_ps=o_ps, g=g, kt=kt, j=j):
                                            MM(o_ps[:, 0:nq], V[:, kt, g * 128:(g + 1) * 128], pt_[:, 0:nq], j == 0, j == len(kts) - 1, [V, pt_], [o_ps])
                                        after = None
                                        if j == len(kts) - 1 and name == "df":
                                            if m == 0:
                                                def after(o_ps=o_ps, h=h):
                                                    stash[h] = fin_a(o_ps, nq)
                                                    return None
                                            else:
                                                def after(o_ps=o_ps, h=h):
                                                    oa = stash[h]
                                                    obb = fin_a(o_ps, nq)

                                                    def part2():
                                                        a1, a2 = d1[h % 2], d2[h % 2]
                                                        fin_b(oa, nq, want_f32=a1)
                                                        fin_b(obb, nq, want_f32=a2)
                                                        STT(a1[:, 0:nq], a2[0:64, 0:nq], lam[:, 2:3], a1[:, 0:nq], ALU.mult, ALU.add, [a1, a2, lam], [a1])
                                                        TT("dve", a2[0:64, 0:nq], a1[:, 0:nq], a1[:, 0:nq], ALU.mult, [a1], [a2])
                                                        rb = RB[0]
                                                        MM(rb[:, 0:nq], ones64[:], a2[:, 0:nq], True, True, [ones64, a2], [rb])
                                                        ACT(d3[:, 0:nq], rb[0:64, 0:nq], AF.Sqrt, [rb], [d3], scale=1.0 / 64, bias=EPS)
                                                        P.op("dve", lambda e, nq=nq: e.reciprocal(out=d3[:, 0:nq], in_=d3[:, 0:nq]), [d3], [d3])
                                                        ob = dob[h % 2]
                                                        STT(ob[:, 0:nq], a1[:, 0:nq], subl[:, 0:1], d3[:, 0:nq], ALU.mult, ALU.mult, [a1, subl, d3], [ob])
                                                        P.dma("sp", oT[bi, h][:, q0:q0 + nq], ob[:, 0:nq], [ob], [oT], "s2_od%d" % (h % 2))
                                                    return part2
                                        elif j == len(kts) - 1:
                                            def after(o_ps=o_ps, h=h):
                                                o = fin_a(o_ps, nq)

                                                def part2():
                                                    ob = fin_b(o, nq)
                                                    P.dma("sp", oT[bi, h][:, q0:q0 + nq], ob[:, 0:nq], [ob], [oT], "s2_o%d" % ((cnt["onb"] - 1) % 2))
                                                return part2
                                        steps.append(dict(qk=qk, ex=ex, pv=pv, after=after))
                            stash = {}
                            run_pipeline(steps, DEFER=6)

                dense(1, "df", 4, 4, 64, 32 ** -0.5, 2)
                dense(2, "gq", 2, 4, 64, 64 ** -0.5, 1)
                dense(3, "ml", 4, 4, 96, 96 ** -0.5, 1)

        def stage3(l, last):
            blocks = ([] if last else [(0, CTX, 1)]) + [(CTX + i * 512, 512, 0) for i in range(S // 512)]
            with P.scope():
                wg = P.sbuf("wg", [128, 8, 4096], BF16)
                wsrc = w_in[l][:, 2464:6560].rearrange("(k p) n -> p k n", p=128)
                for k in range(8):
                    P.dma("pool", wg[:, k, :], wsrc[:, k, :], [], [wg], "s3_wg")
                wb = P.sbuf("wb", [128, 8, D], BF16)
                P.dma("pool", wb[:], w_branch[l].rearrange("(j p) n -> p j n", p=128), [], [wb], "s3_wb")
                wo = P.sbuf("wo", [128, 8, D], BF16)
                P.dma("pool", wo[:], w_out[l].rearrange("(k p) n -> p k n", p=128), [], [wo], "s3_wo")
                rw = P.sbuf("rw", [128, 8, NE], F32)
                P.dma("sp", rw[:], router_w[l].rearrange("(k p) n -> p k n", p=128), [], [rw], "s3_rw")
                rbias = P.sbuf("rbias", [128, NE], F32)
                P.dma("sp", rbias[:], router_b[l].partition_broadcast(128), [], [rbias], "s3_rb")
                G1 = P.sbuf("G1", [128, D], F32)
                A2 = P.sbuf("A2", [128, D], F32)
                B2 = P.sbuf("B2", [128, D], F32)
                lg = P.sbuf("lg", [128, D], F32)
                lb = P.sbuf("lb", [128, D], F32)
                P.dma("sp", lg[:], ln1_g[l].partition_broadcast(128), [], [lg], "s3_lg")
                P.dma("sp", lb[:], ln1_b[l].partition_broadcast(128), [], [lb], "s3_lb")
                hTb = P.sbuf("hTb", [128, 8, 512], BF16)
                ot = [P.sbuf("ot%d" % i, [128, 2, 512], BF16) for i in range(4)]
                yT = P.sbuf("yT", [128, 8, 512], BF16)
                sig = [P.sbuf("sig%d" % i, [128, 512], F32) for i in range(2)]
                term = P.sbuf("term", [128, 512], F32)
                yacc = P.sbuf("yacc", [128, 512], F32)
                xs = P.sbuf("s3_xs", [128, D], F32)
                rr = P.sbuf("s3_r", [128, D], F32)
                tmp = P.sbuf("s3_tmp", [128, D], F32)
                x1 = P.sbuf("s3_x1", [128, D], F32)
                h2 = P.sbuf("s3_h2", [128, D], F32)
                h2T = P.sbuf("s3_h2T", [128, 8, 128], F32)
                h2b = P.sbuf("s3_h2b", [128, D], BF16)
                tri = P.sbuf("s3_tri", [128, 128], F32)
                onesm = P.sbuf("s3_onesm", [128, 128], F32)
                P.dma("sp", tri[:], tri_in, [], [tri], "s3_tri")
                MEMSET("dve", onesm[:], 1.0, [onesm])
                cnt = P.sbuf("s3_cnt", [128, NE], F32)
                MEMSET("dve", cnt[:], 0.0, [cnt])
                rnk = P.sbuf("s3_rnk", [128, NE], F32)
                st6 = P.sbuf("s3_st6", [128, 2, 6], F32)
                mv = P.sbuf("s3_mv", [128, 2], F32)
                lgt = P.sbuf("s3_lgt", [128, NE], F32)
                m8 = P.sbuf("s3_m8", [128, 8], F32)
                msk = P.sbuf("s3_msk", [128, NE], F32)
                sm = P.sbuf("s3_sm", [128, 2], F32)
                comb = P.sbuf("s3_comb", [128, 128], F32)
                MEMSET("dve", comb[:], 0.0, [comb])
                pG = [P.psum("pG%d" % i, [128, 512], F32) for i in range(2)]
                pB = [P.psum("pB%d" % i, [128, 512], F32) for i in range(2)]
                pO = [P.psum("pO%d" % i, [128, 512], F32) for i in range(2)]
                pT_ = [P.psum("pT%d" % i, [128, 4, 128], F32) for i in range(2)]
                cur_s = None
                n = 0
                for (t0, nt, s) in blocks:
                    if s != cur_s:
                        cur_s = s
                        P.dma("sp", G1[:], MOD[s, :, 2048:3072], [MOD], [G1], "s3_m0")
                        P.dma("sp", A2[:], MOD[s, :, 4096:5120], [MOD], [A2], "s3_m1")
                        P.dma("sp", B2[:], MOD[s, :, 3072:4096], [MOD], [B2], "s3_m2")
                    ntile = nt // 128
                    for ti in range(ntile):
                        P.dma("sp", hTb[:, :, ti * 128:(ti + 1) * 128], hTs[t0 // 128 + ti], [hTs], [hTb], "s3_hT")
                    for i in range(4):
                        P.dma("sp", ot[i][:, :, 0:nt], oT[i].rearrange("(j hh) d t -> (hh d) j t", hh=2)[:, :, t0:t0 + nt], [oT], [ot[i]], "s3_ot%d" % i)
                    for c in range(8):
                        for i in range(4):
                            g_, b_, sg = pG[n % 2], pB[n % 2], sig[n % 2]
                            n += 1
                            for k in range(8):
                                MM(g_[:, 0:nt], wg[:, k, i * 1024 + c * 128:i * 1024 + (c + 1) * 128], hTb[:, k, 0:nt], k == 0, k == 7, [wg, hTb], [g_])
                            ACT(sg[:, 0:nt], g_[:, 0:nt], AF.Sigmoid, [g_], [sg])
                            for j in range(2):
                                MM(b_[:, 0:nt], wb[:, i * 2 + j, c * 128:(c + 1) * 128], ot[i][:, j, 0:nt], j == 0, j == 1, [wb, ot[i]], [b_])
                            if i == 0:
                                TT("dve", yacc[:, 0:nt], sg[:, 0:nt], b_[:, 0:nt], ALU.mult, [sg, b_], [yacc])
                            else:
                                TT("dve", term[:, 0:nt], sg[:, 0:nt], b_[:, 0:nt], ALU.mult, [sg, b_], [term])
                                if i < 3:
                                    TT("pool", yacc[:, 0:nt], yacc[:, 0:nt], term[:, 0:nt], ALU.add, [yacc, term], [yacc])
                                else:
                                    TT("pool", yT[:, c, 0:nt], yacc[:, 0:nt], term[:, 0:nt], ALU.add, [yacc, term], [yT])
                    for ti in range(ntile):
                        tg = t0 // 128 + ti
                        rows = slice(tg * 128, (tg + 1) * 128)
                        if l == 0:
                            src = ctx_in[rows, :] if tg < NCT else x_in[(tg - NCT) * 128:(tg - NCT + 1) * 128, :]
                            P.dma("sp", xs[:], src, [], [xs], "s3_x")
                        else:
                            P.dma("sp", xs[:], xB[rows, :], [xB], [xs], "s3_x")
                        for hf_ in range(2):
                            for c in range(8):
                                MM(pO[hf_][:], yT[:, c, ti * 128:(ti + 1) * 128], wo[:, c, hf_ * 512:(hf_ + 1) * 512], c == 0, c == 7, [yT, wo], [pO[hf_]])
                            TT("dve", tmp[:, hf_ * 512:(hf_ + 1) * 512], pO[hf_][:], G1[:, hf_ * 512:(hf_ + 1) * 512], ALU.mult, [pO[hf_], G1], [tmp])
                        STT(rr[:], xs[:], ALPHA, tmp[:], ALU.mult, ALU.add, [xs, tmp], [rr])
                        layer_norm(rr, tmp, lg, lb, st6, mv, x1)
                        P.dma("sp", xA[rows, :], x1[:], [x1], [xA], "s3_xA")
                        TT("pool", tmp[:], x1[:], A2[:], ALU.mult, [x1, A2], [tmp])
                        TT("dve", h2[:], tmp[:], B2[:], ALU.add, [tmp, B2], [h2])
                        for hf_ in range(2):
                            for k in range(4):
                                kk = hf_ * 4 + k
                                TRF(pT_[hf_][:, k, :], h2[:, kk * 128:(kk + 1) * 128], [h2], [pT_[hf_]])
                            CP("act", h2T[:, hf_ * 4:(hf_ + 1) * 4, :], pT_[hf_][:], [pT_[hf_]], [h2T])
                        CP("pool", h2b[:], h2[:], [h2], [h2b])
                        P.dma("sp", h2bS[rows, :], h2b[:], [h2b], [h2bS], "s3_h2b")
                        pl = pB[0]
                        for k in range(8):
                            MM(pl[:, 0:NE], h2T[:, k, :], rw[:, k, :], k == 0, k == 7, [h2T, rw], [pl])
                        TT("dve", lgt[:], pl[:, 0:NE], rbias[:], ALU.add, [pl, rbias], [lgt])
                        P.op("dve", lambda e: e.max(out=m8[:], in_=lgt[:]), [lgt], [m8])
                        TS("dve", msk[:], lgt[:], m8[:, 3:4], ALU.is_ge, [lgt, m8], [msk])
                        TS("dve", sm[:, 0:1], m8[:, 0:1], -1.0, ALU.mult, [m8], [sm])
                        ACT(lgt[:], lgt[:], AF.Exp, [lgt, sm], [lgt], bias=sm[:, 0:1])
                        TT("dve", lgt[:], lgt[:], msk[:], ALU.mult, [lgt, msk], [lgt])
                        P.op("dve", lambda e: e.tensor_reduce(out=sm[:, 1:2], in_=lgt[:], axis=AX.X, op=ALU.add), [lgt], [sm])
                        P.op("dve", lambda e: e.reciprocal(out=sm[:, 1:2], in_=sm[:, 1:2]), [sm], [sm])
                        TS("dve", comb[:, 0:NE], lgt[:], sm[:, 1:2], ALU.mult, [lgt, sm], [comb])
                        P.dma("sp", combS[rows, :], comb[:], [comb], [combS], "s3_cb")
                        pr = pB[1]
                        MM(pr[:, 0:NE], tri[:], msk[:], True, True, [tri, msk], [pr])
                        MM(pr[:, 64:64 + NE], onesm[:], msk[:], True, True, [onesm, msk], [pr])
                        TT("dve", rnk[:], pr[:, 0:NE], cnt[:], ALU.add, [pr, cnt], [rnk])
                        TT("dve", cnt[:], pr[:, 64:64 + NE], cnt[:], ALU.add, [pr, cnt], [cnt])
                        P.dma("sp", mskS[rows, :], msk[:], [msk], [mskS], "s3_mk")
                        P.dma("sp", rankS[rows, :], rnk[:], [rnk], [rankS], "s3_rk")

                P.dma("sp", cntS[:], cnt[:], [cnt], [cntS], "s3_cnt")

        def stage4(l, last):
            t_first = NCT if last else 0
            Tm = (NT - t_first) * 128
            nslot = (4 * Tm) // 512 + NE
            maxm = (Tm + 511) // 512
            w1rows = exp_w1.rearrange("l e d (g n) -> (l e d g) n", g=2)
            w2rows = exp_w2.rearrange("l e d n -> (l e d) n")
            b1rows = exp_b1.rearrange("l e (g n) -> (l e g) n", g=2)
            b2rows = exp_b2.rearrange("l e n -> (l e) n")
            with P.scope():
                posu = P.sbuf("posu", [128, NT, 4], U32)
                ck = P.sbuf("ck", [128, NT, 4], F32)
                idx = P.sbuf("idx", [128, 3, nslot, 8], U32)
                es = P.sbuf("a_es", [128, nslot], F32)
                bidx = P.sbuf("bidx", [128, 3, nslot], U32)
                with P.scope():
                    cnt = P.sbuf("a_cnt", [128, NE], F32)
                    P.dma("sp", cnt[:], cntS[:], [cntS], [cnt], "a_cnt")
                    pad = P.sbuf("a_pad", [128, NE], F32)
                    tmpe = P.sbuf("a_tmpe", [128, NE], F32)
                    TS("dve", pad[:], cnt[:], 0.0, ALU.is_gt, [cnt], [pad], s2=512.0, op1=ALU.mult)
                    for m in range(1, maxm):
                        TS("dve", tmpe[:], cnt[:], 512.0 * m, ALU.is_gt, [cnt], [tmpe], s2=512.0, op1=ALU.mult)
                        TT("dve", pad[:], pad[:], tmpe[:], ALU.add, [pad, tmpe], [pad])
                    start = P.sbuf("a_start", [128, NE], F32)
                    MEMSET("dve", start[:], 0.0, [start])
                    for e in range(1, NE):
                        TT("dve", start[:, e:e + 1], start[:, e - 1:e], pad[:, e - 1:e], ALU.add, [start, pad], [start])
                    endt = P.sbuf("a_end", [128, NE], F32)
                    TT("dve", endt[:], start[:], pad[:], ALU.add, [start, pad], [endt])
                    sst = P.sbuf("a_sst", [128, nslot], F32)
                    P.dma("sp", sst[:], sst_in[:, 0:nslot], [], [sst], "a_sst")
                    MEMSET("dve", es[:], 0.0, [es])
                    for e in range(NE):
                        STT(es[:], sst[:], endt[:, e:e + 1], es[:], ALU.is_ge, ALU.add, [sst, endt, es], [es])
                    TS("dve", es[:], es[:], float(NE - 1), ALU.min, [es], [es])
                    iok = P.sbuf("a_iok", [128, 8], F32)
                    P.dma("sp", iok[:], iok_in, [], [iok], "a_iok")
                    idf_ = P.sbuf("a_idf", [128, 3, nslot, 8], F32)
                    bdf_ = P.sbuf("a_bdf", [128, 3, nslot], F32)
                    for k in range(8):
                        TS("dve", idf_[:, 0, :, k], es[:], float(D), ALU.mult, [es, iok], [idf_], s2=iok[:, k:k + 1], op1=ALU.add)
                    TS("dve", idf_[:, 0, :, :], idf_[:, 0, :, :], float(l * NE * D), ALU.add, [idf_], [idf_])
                    TS("dve", idf_[:, 1, :, :], idf_[:, 0, :, :], 2.0, ALU.mult, [idf_], [idf_])
                    TS("dve", idf_[:, 2, :, :], idf_[:, 0, :, :], 2.0, ALU.mult, [idf_], [idf_], s2=1.0, op1=ALU.add)
                    CP("dve", idx[:], idf_[:], [idf_], [idx])
                    TS("dve", bdf_[:, 0, :], es[:], float(l * NE), ALU.add, [es], [bdf_])
                    TS("dve", bdf_[:, 1, :], bdf_[:, 0, :], 2.0, ALU.mult, [bdf_], [bdf_])
                    TS("dve", bdf_[:, 2, :], bdf_[:, 0, :], 2.0, ALU.mult, [bdf_], [bdf_], s2=1.0, op1=ALU.add)
                    CP("dve", bidx[:], bdf_[:], [bdf_], [bidx])
                    mk = [P.sbuf("a_mk%d" % i, [128, NE], F32) for i in range(2)]
                    rk = [P.sbuf("a_rk%d" % i, [128, NE], F32) for i in range(2)]
                    cb = [P.sbuf("a_cb%d" % i, [128, NE], F32) for i in range(2)]
                    hb_ = [P.sbuf("a_hb%d" % i, [128, D], BF16) for i in range(2)]
                    p1 = P.sbuf("a_p1", [128, NE], F32)
                    m8 = P.sbuf("a_m8", [128, 8], F32)
                    pf = P.sbuf("a_pf", [128, 4], F32)
                    for t in range(t_first, NT):
                        i = t % 2
                        rows = slice(t * 128, (t + 1) * 128)
                        P.dma("sp", mk[i][:], mskS[rows, :], [mskS], [mk[i]], "a_l0%d" % i)
                        P.dma("sp", rk[i][:], rankS[rows, :], [rankS], [rk[i]], "a_l1%d" % i)
                        P.dma("sp", cb[i][:], combS[rows, 0:NE], [combS], [cb[i]], "a_l2%d" % i)
                        P.dma("sp", hb_[i][:], h2bS[rows, :], [h2bS], [hb_[i]], "a_l3%d" % i)
                        TT("dve", p1[:], rk[i][:], start[:], ALU.add, [rk[i], start], [p1])
                        STT(p1[:], p1[:], 1.0, mk[i][:], ALU.add, ALU.mult, [p1, mk[i]], [p1])
                        P.op("dve", lambda e: e.max(out=m8[:], in_=p1[:]), [p1], [m8])
                        TS("dve", pf[:], m8[:, 0:4], -1.0, ALU.add, [m8], [pf])
                        CP("dve", posu[:, t, :], pf[:], [pf], [posu])
                        for k in range(4):
                            STT(tmpe[:], p1[:], m8[:, k:k + 1], cb[i][:], ALU.is_equal, ALU.mult, [p1, m8, cb[i]], [tmpe])
                            P.op("dve", lambda e, k=k, t=t: e.tensor_reduce(out=ck[:, t, k:k + 1], in_=tmpe[:], axis=AX.X, op=ALU.add), [tmpe], [ck])
                        for k in range(4):
                            P._emit("pool", lambda e, k=k, t=t, i=i: e.indirect_dma_start(
                                out=XsT, out_offset=bass.IndirectOffsetOnAxis(ap=posu[:, t, k:k + 1], axis=0), in_=hb_[i][:], in_offset=None),
                                [hb_[i], posu], XsB[0:nslot], dma_key="a_sc", nowaw="a_sc")
                with P.scope():
                    NR = 5
                    ringt = [P.sbuf("ring%d" % i, [128, 8, D], BF16) for i in range(NR)]
                    ring = [[Buf("ring%d_%d" % (i, k), ringt[i].t[:, k, :]) for k in range(8)] for i in range(NR)]
                    bt = [[P.sbuf("bias%d_%d" % (i, g), [128, D], BF16) for g in range(1)] for i in range(2)]
                    b1r = P.sbuf("b1r", [128, 2 * D], F32)
                    MEMSET("dve", b1r[:], 0.0, [b1r])
                    P.dma("sp", b1r[0:NE, :], exp_b1[l], [], [b1r], "b_b1")
                    b1T = P.sbuf("b1T", [128, 16, NE], F32)
                    ioe = P.sbuf("ioe", [128, NE], F32)
                    P.dma("sp", ioe[:], ioe_in, [], [ioe], "b_ioe")
                    oh = [P.sbuf("oh%d" % i, [128, NE], F32) for i in range(2)]
                    b1x = [P.sbuf("b1x%d" % i, [128, 16, NE], F32) for i in range(2)]
                    b1s = [P.sbuf("b1s%d" % i, [128, 16], F32) for i in range(2)]
                    loads = []
                    for sl in range(nslot):
                        loads += [(sl, 1), (sl, 2), (sl, 0)]
                    issued = [0]

                    def ensure(n):
                        while issued[0] <= n and issued[0] < len(loads):
                            i = issued[0]
                            sl, part = loads[i]
                            rows_ap = w2rows if part == 0 else w1rows
                            for k in range(8):
                                b = ring[i % NR][k]
                                P._emit("pool", lambda e, b=b, sl=sl, part=part, k=k, rows_ap=rows_ap: e.indirect_dma_start(
                                    out=b[:], out_offset=None, in_=rows_ap,
                                    in_offset=bass.IndirectOffsetOnAxis(ap=idx[:, part, sl, k:k + 1], axis=0)),
                                    [idx], [b], dma_key="b_w%d_%d" % (i % NR, k))
                            if part == 1:
                                for g, rap in ((0, b2rows),):
                                    b = bt[sl % 2][g]
                                    P._emit("pool", lambda e, b=b, sl=sl, g=g, rap=rap: e.indirect_dma_start(
                                        out=b[:], out_offset=None, in_=rap,
                                        in_offset=bass.IndirectOffsetOnAxis(ap=bidx[:, g, sl:sl + 1], axis=0)),
                                        [bidx], [b], dma_key="b_b%d_%d" % (sl % 2, g))
                            issued[0] += 1

                    onerow = P.sbuf("onerow", [128, 512], BF16)
                    MEMSET("dve", onerow[:], 0.0, [onerow])
                    MEMSET("dve", onerow[0:1, :], 1.0, [onerow])
                    xts = [P.sbuf("b_xt%d" % i, [128, 4, D], BF16) for i in range(2)]
                    h2Ts_ = [P.sbuf("b_h2T%d" % i, [128, 8, 512], BF16) for i in range(2)]
                    actT = [P.sbuf("actT%d" % i, [128, 8, 512], BF16) for i in range(2)]
                    gt = [P.sbuf("gt%d" % i, [128, 512], F32) for i in range(2)]
                    sg = [P.sbuf("sg%d" % i, [128, 512], F32) for i in range(2)]
                    gs = [P.sbuf("gs%d" % i, [128, 512], F32) for i in range(2)]
                    up = [P.sbuf("up%d" % i, [128, 512], F32) for i in range(2)]
                    ysb = [P.sbuf("ysb%d" % i, [128, D], F32) for i in range(2)]
                    pq = [P.psum("b_pq%d" % i, [128, 512], F32) for i in range(4)]
                    py = [P.psum("b_py%d" % i, [128, 2, 512], F32) for i in range(1)]
                    ptr = [P.psum("b_ptr%d" % i, [128, 8, 128], BF16) for i in range(2)]
                    nq = [0, 0, 0]
                    for j in range(16):
                        pp = pq[j % 4]
                        TRF(pp[:, 0:128], b1r[:, j * 128:(j + 1) * 128], [b1r], [pp])
                        CP("act", b1T[:, j, :], pp[:, 0:NE], [pp], [b1T])
                    TS("dve", b1T[:, 8:16, :], b1T[:, 8:16, :], 1.0, ALU.add, [b1T], [b1T])

                    def load_x(sl):
                        xt = xts[sl % 2]
                        P.dma("sp", xt[:], XsB[sl].t.rearrange("(n p) d -> p n d", p=128), [XsB[sl]], [xt], "b_xt%d" % (sl % 2))

                    def trans_x(sl):
                        xt, h2T = xts[sl % 2], h2Ts_[sl % 2]
                        for ti in range(4):
                            pb = ptr[nq[2] % 2]
                            nq[2] += 1
                            for k in range(8):
                                TRB(pb[:, k, :], xt[:, ti, k * 128:(k + 1) * 128], [xt], [pb])
                            CP("act", h2T[:, :, ti * 128:(ti + 1) * 128], pb[:], [pb], [h2T])

                    for sl in range(nslot):
                        base = sl * 3
                        ensure(base + 4)
                        wgt, wup, w2 = ring[base % NR], ring[(base + 1) % NR], ring[(base + 2) % NR]
                        b2t = bt[sl % 2][0]
                        oh_, bx_, bs_ = oh[sl % 2], b1x[sl % 2], b1s[sl % 2]
                        TS("dve", oh_[:], ioe[:], es[:, sl:sl + 1], ALU.is_equal, [ioe, es], [oh_])
                        TT("dve", bx_[:], b1T[:], oh_[:].unsqueeze(1).broadcast_to([128, 16, NE]), ALU.mult, [b1T, oh_], [bx_])
                        P.op("dve", lambda e, bx_=bx_, bs_=bs_: e.tensor_reduce(out=bs_[:], in_=bx_[:], axis=AX.X, op=ALU.add), [bx_], [bs_])
                        if sl == 0:
                            load_x(0)
                            trans_x(0)
                        if sl + 1 < nslot:
                            load_x(sl + 1)
                        h2T = h2Ts_[sl % 2]
                        aT = actT[sl % 2]
                        for j in range(8):
                            pg_, pu_ = pq[nq[0] % 4], pq[(nq[0] + 1) % 4]
                            i2 = (nq[0] // 2) % 2
                            nq[0] += 2
                            for k in range(8):
                                MM(pg_[:], wgt[k][:, j * 128:(j + 1) * 128], h2T[:, k, :], k == 0, k == 7, [wgt[k], h2T], [pg_])
                            for k in range(8):
                                MM(pu_[:], wup[k][:, j * 128:(j + 1) * 128], h2T[:, k, :], k == 0, k == 7, [wup[k], h2T], [pu_])
                            TS("dve", gt[i2][:], pg_[:], bs_[:, j:j + 1], ALU.add, [pg_, bs_], [gt[i2]], s2=7.0, op1=ALU.min)
                            ACT(sg[i2][:], gt[i2][:], AF.Sigmoid, [gt[i2]], [sg[i2]], scale=1.702)
                            TT("dve", gs[i2][:], gt[i2][:], sg[i2][:], ALU.mult, [gt[i2], sg[i2]], [gs[i2]])
                            TS("dve", up[i2][:], pu_[:], bs_[:, 8 + j:9 + j], ALU.add, [pu_, bs_], [up[i2]], s2=-6.0, op1=ALU.max)
                            STT(aT[:, j, :], up[i2][:], 8.0, gs[i2][:], ALU.min, ALU.mult, [up[i2], gs[i2]], [aT])
                        ensure(base + 5)
                        if sl + 1 < nslot:
                            trans_x(sl + 1)
                        for ti in range(4):
                            yp = py[0]
                            ys_ = ysb[nq[1] % 2]
                            nq[1] += 1
                            for hf_ in range(2):
                                for j in range(8):
                                    MM(yp[:, hf_, :], aT[:, j, ti * 128:(ti + 1) * 128], w2[j][:, hf_ * 512:(hf_ + 1) * 512], j == 0, False, [aT, w2[j]], [yp])
                                MM(yp[:, hf_, :], onerow[:, 0:128], b2t[:, hf_ * 512:(hf_ + 1) * 512], False, True, [onerow, b2t], [yp])
                            CP("act", ys_[:], yp[:].rearrange("p a b -> p (a b)"), [yp], [ys_])
                            P.dma("sp", YsT[sl * 512 + ti * 128:sl * 512 + (ti + 1) * 128, :], ys_[:], [ys_], [YsB[sl]], "b_ys%d" % ((nq[1] - 1) % 2))
                with P.scope():
                    G2 = [P.sbuf("G2_%d" % s_, [128, D], F32) for s_ in range(1 if last else 2)]
                    for s_ in range(len(G2)):
                        P.dma("sp", G2[s_][:], MOD[s_, :, 5120:6144], [MOD], [G2[s_]], "c_g2%d" % s_)
                    lg = P.sbuf("c_lg", [128, D], F32)
                    lb = P.sbuf("c_lb", [128, D], F32)
                    P.dma("sp", lg[:], ln2_g[l].partition_broadcast(128), [], [lg], "c_lg")
                    P.dma("sp", lb[:], ln2_b[l].partition_broadcast(128), [], [lb], "c_lb")
                    gth = [[P.sbuf("c_g%d_%d" % (i, k), [128, D], F32) for k in range(4)] for i in range(2)]
                    xs = [P.sbuf("c_xs%d" % i, [128, D], F32) for i in range(2)]
                    acc = [P.sbuf("c_acc%d" % i, [128, D], F32) for i in range(2)]
                    st6 = P.sbuf("c_st6", [128, 2, 6], F32)
                    mv = P.sbuf("c_mv", [128, 2], F32)
                    for t in range(t_first, NT):
                        i = t % 2
                        rows = slice(t * 128, (t + 1) * 128)
                        s_ = 1 if t < NCT else 0
                        for k in range(4):
                            P._emit("pool", lambda e, k=k, t=t, i=i: e.indirect_dma_start(
                                out=gth[i][k][:], out_offset=None, in_=YsT,
                                in_offset=bass.IndirectOffsetOnAxis(ap=posu[:, t, k:k + 1], axis=0)),
                                YsB[0:nslot] + [posu], [gth[i][k]], dma_key="c_g%d_%d" % (i, k))
                        P.dma("sp", xs[i][:], xA[rows, :], [xA], [xs[i]], "c_x%d" % i)
                        a = acc[i]
                        TS("dve", a[:], gth[i][0][:], ck[:, t, 0:1], ALU.mult, [gth[i][0], ck], [a])
                        for k in range(1, 4):
                            STT(a[:], gth[i][k][:], ck[:, t, k:k + 1], a[:], ALU.mult, ALU.add, [gth[i][k], ck, a], [a])
                        TT("dve", a[:], a[:], G2[s_][:], ALU.mult, [a, G2[s_]], [a])
                        STT(a[:], xs[i][:], ALPHA, a[:], ALU.mult, ALU.add, [xs[i], a], [a])
                        layer_norm(a, xs[i], lg, lb, st6, mv, a, geng="dve")
                        if last:
                            P.dma("sp", out_ap[(t - NCT) * 128:(t - NCT + 1) * 128, :], a[:], [a], [], "c_out%d" % i)
                        else:
                            P.dma("sp", xB[rows, :], a[:], [a], [xB], "c_out%d" % i)

        for l in range(L):
            last = (l == L - 1)
            stage0(l)
            stage1(l, last)
            stage2(l, last)
            stage3(l, last)
            stage4(l, last)
        P.flush()
    return nc, NCFG


_CACHE = {}


def make_inputs(inputs, S, L, NE, n_cores):
    per_q, cfg_list = na_structure(S)
    ncfg = len(cfg_list)
    rpb = np.asarray(inputs["na_rpb"], np.float32)
    na_bias = np.stack([rpb[:, :, a, b] for (a, b, _) in cfg_list], axis=1).astype(np.float32)
    na_mask = np.stack([m for (_, _, m) in cfg_list], axis=0).astype(np.float32)
    shared = {
        "w_ada": inputs["w_ada"], "b_ada": inputs["b_ada"], "w_in": inputs["w_in"],
        "na_bias": np.ascontiguousarray(na_bias), "na_mask": na_mask,
        "df_lam": np.asarray(inputs["df_lam"]).reshape(L, 128), "df_subln": inputs["df_subln"],
        "gq_qnorm": inputs["gq_qnorm"], "gq_knorm": inputs["gq_knorm"],
        "ml_qa_norm": inputs["ml_qa_norm"], "ml_wq_b": inputs["ml_wq_b"],
        "ml_kva_norm": inputs["ml_kva_norm"], "ml_wkv_b": inputs["ml_wkv_b"],
        "w_branch": np.asarray(inputs["w_branch"]).reshape(L, 1024, D), "w_out": inputs["w_out"],
        "ln1_g": inputs["ln1_g"], "ln1_b": inputs["ln1_b"], "ln2_g": inputs["ln2_g"], "ln2_b": inputs["ln2_b"],
        "router_w": inputs["router_w"], "router_b": inputs["router_b"],
        "exp_w1": inputs["exp_w1"], "exp_b1": inputs["exp_b1"], "exp_w2": inputs["exp_w2"], "exp_b2": inputs["exp_b2"],
        "idn": np.eye(128, dtype=np.float32), "rope": rope_tables(S),
        "tri": np.triu(np.ones((128, 128), np.float32), 1),
        "slotstart": np.tile((512.0 * np.arange((4 * (CTX + S) + 511) // 512 + NE, dtype=np.float32))[None, :], (128, 1)),
        "iotak": (np.arange(8, dtype=np.float32)[None, :] * 128 + np.arange(128, dtype=np.float32)[:, None]),
        "iotae": np.tile(np.arange(NE, dtype=np.float32)[None, :], (128, 1)),
    }
    shared = {k: np.ascontiguousarray(np.asarray(v, np.float32)) for k, v in shared.items()}
    maps = []
    for b in range(n_cores):
        m = dict(shared)
        m["x"] = np.ascontiguousarray(np.asarray(inputs["x"][b], np.float32))
        m["ctx"] = np.ascontiguousarray(np.asarray(inputs["ctx"][b], np.float32))
        m["c2"] = np.ascontiguousarray(np.stack([np.asarray(inputs["c"][b], np.float32), np.asarray(inputs["c_ctx"], np.float32)], axis=0))
        maps.append(m)
    return maps


def kernel(**inputs):
    x = np.asarray(inputs["x"])
    B, S, _ = x.shape
    L = int(np.asarray(inputs["w_ada"]).shape[0])
    NE = int(np.asarray(inputs["router_w"]).shape[-1])
    key = (S, L, NE)
    if key not in _CACHE:
        _CACHE[key] = build(S, L, NE)[0]
    nc = _CACHE[key]
    maps = make_inputs(inputs, S, L, NE, B)
    res = run_bass_kernel_spmd(nc, maps, core_ids=list(range(B)))
    return np.stack([np.asarray(r["out"], np.float32) for r in res.results], axis=0)
```
